# Optimizing a Trainium2 kernel written in Bass

```python
import math
import jax, jax.numpy as jnp
from jax import lax
import numpy as np

D_MODEL = 2048
BATCH = 4
SEQ = 8192
DEPTH = 1
DEC_BATCH = 1
DEC_SEQ = 16384
PAST_LEN = 128

GRID_W = 64
Q_BLOCK = 128
ROPE_THETA = 10000.0
A_HEADS = 16
A_Q_RANK = 512
A_KV_RANK = 512
A_NOPE = 128
A_ROPE = 64
A_V = 128
B_HEADS = 16
B_KV_HEADS = 4
B_GROUP = B_HEADS // B_KV_HEADS
B_HEAD_DIM = 128
N_GROUPS = 8
EXPERTS_PER_GROUP = 8
N_EXPERTS = N_GROUPS * EXPERTS_PER_GROUP
TOP_K = 2
D_EXPERT = 512
EXPERT_BLOCK = 128
RMS_EPS = 1e-6
LN_EPS = 1e-5
DEEPNORM_ALPHA = (2 * DEPTH) ** 0.25
DEEPNORM_BETA = (8 * DEPTH) ** -0.25
IN_SPLITS = (A_Q_RANK, A_KV_RANK, A_ROPE, B_HEADS * B_HEAD_DIM, B_KV_HEADS * B_HEAD_DIM,
             B_KV_HEADS * B_HEAD_DIM, D_MODEL, D_MODEL)
D_IN = sum(IN_SPLITS)
IN_OFFSETS = tuple(np.cumsum(IN_SPLITS)[:-1].tolist())

kernel_name = 'hybrid_mla_axialgqa_hiermoe_encoder'


def _rms_norm(x, g):
    xf = x.astype(jnp.float32)
    y = xf * lax.rsqrt(jnp.mean(xf * xf, axis=-1, keepdims=True) + RMS_EPS)
    return (y * g.astype(jnp.float32)).astype(x.dtype)


def _layer_norm(x, g, b):
    xf = x.astype(jnp.float32)
    mu = jnp.mean(xf, axis=-1, keepdims=True)
    xc = xf - mu
    var = jnp.mean(xc * xc, axis=-1, keepdims=True)
    y = xc * lax.rsqrt(var + LN_EPS) * g.astype(jnp.float32) + b.astype(jnp.float32)
    return y.astype(x.dtype)


def _rope_angles(pos, dim):
    inv = ROPE_THETA ** (-jnp.arange(0, dim, 2, dtype=jnp.float32) / dim)
    return pos.astype(jnp.float32)[:, None] * inv[None, :]


def _apply_rope(x, ang):
    d2 = x.shape[-1] // 2
    shape = (1, ang.shape[0]) + (1,) * (x.ndim - 3) + (d2,)
    cos = jnp.cos(ang).reshape(shape)
    sin = jnp.sin(ang).reshape(shape)
    xf = x.astype(jnp.float32)
    x1, x2 = xf[..., :d2], xf[..., d2:]
    return jnp.concatenate([x1 * cos - x2 * sin, x2 * cos + x1 * sin], axis=-1).astype(x.dtype)


def _axial_rope(x, ang_row, ang_col):
    h = x.shape[-1] // 2
    return jnp.concatenate([_apply_rope(x[..., :h], ang_row), _apply_rope(x[..., h:], ang_col)], axis=-1)


def _to_blocks(x):
    b, s = x.shape[:2]
    return jnp.moveaxis(x.reshape((b, s // Q_BLOCK, Q_BLOCK) + x.shape[2:]), 1, 0)


def _from_blocks(o):
    nb, b, q = o.shape[:3]
    return jnp.moveaxis(o, 0, 1).reshape((b, nb * q) + o.shape[3:])


def _mla_branch(c_q, c_kv, k_rope_raw, ang, q_norm_g, kv_norm_g, w_uq, w_ukv, w_o):
    b, s, _ = c_q.shape
    q = (_rms_norm(c_q, q_norm_g) @ w_uq).reshape(b, s, A_HEADS, A_NOPE + A_ROPE)
    q_nope = q[..., :A_NOPE]
    q_rope = _apply_rope(q[..., A_NOPE:], ang)
    kv = (_rms_norm(c_kv, kv_norm_g) @ w_ukv).reshape(b, s, A_HEADS, A_NOPE + A_V)
    k_nope, v = kv[..., :A_NOPE], kv[..., A_NOPE:]
    k_rope = _apply_rope(k_rope_raw, ang)
    scale = (A_NOPE + A_ROPE) ** -0.5

    def attend(blk):
        qn, qr = blk
        sc = (jnp.einsum('bqhd,bkhd->bhqk', qn, k_nope, preferred_element_type=jnp.float32)
              + jnp.einsum('bqhd,bkd->bhqk', qr, k_rope, preferred_element_type=jnp.float32))
        p = jax.nn.softmax(sc * scale, axis=-1).astype(v.dtype)
        return jnp.einsum('bhqk,bkhd->bqhd', p, v)

    o = _from_blocks(lax.map(attend, (_to_blocks(q_nope), _to_blocks(q_rope))))
    return o.reshape(b, s, A_HEADS * A_V) @ w_o


def _gqa_branch(q, k, v, ang_row, ang_col, q_norm_g, k_norm_g, w_o):
    b, s, _ = q.shape
    q = q.reshape(b, s, B_KV_HEADS, B_GROUP, B_HEAD_DIM)
    k = k.reshape(b, s, B_KV_HEADS, B_HEAD_DIM)
    v = v.reshape(b, s, B_KV_HEADS, B_HEAD_DIM)
    q = _axial_rope(_rms_norm(q, q_norm_g), ang_row, ang_col)
    k = _axial_rope(_rms_norm(k, k_norm_g), ang_row, ang_col)
    scale = B_HEAD_DIM ** -0.5

    def attend(qb):
        sc = jnp.einsum('bqngd,bknd->bngqk', qb, k, preferred_element_type=jnp.float32)
        p = jax.nn.softmax(sc * scale, axis=-1).astype(v.dtype)
        return jnp.einsum('bngqk,bknd->bqngd', p, v)

    o = _from_blocks(lax.map(attend, _to_blocks(q)))
    return o.reshape(b, s, B_HEADS * B_HEAD_DIM) @ w_o


def _grouped_experts(hf, eid, wt, w_gate, w_up, w_down):
    n, d = hf.shape
    a = eid.shape[0]
    tok = jnp.arange(a, dtype=jnp.int32) // TOP_K
    order = jnp.argsort(eid)
    e_sorted = eid[order]
    counts = jnp.bincount(eid, length=N_EXPERTS)
    starts = jnp.cumsum(counts) - counts
    padded = ((counts + EXPERT_BLOCK - 1) // EXPERT_BLOCK) * EXPERT_BLOCK
    pad_end = jnp.cumsum(padded)
    pad_start = pad_end - padded
    dest = pad_start[e_sorted] + (jnp.arange(a, dtype=jnp.int32) - starts[e_sorted])
    p_rows = ((a + EXPERT_BLOCK - 1) // EXPERT_BLOCK) * EXPERT_BLOCK + N_EXPERTS * EXPERT_BLOCK
    n_blk = p_rows // EXPERT_BLOCK
    buf_tok = jnp.zeros((p_rows,), jnp.int32).at[dest].set(tok[order])
    buf_w = jnp.zeros((p_rows,), hf.dtype).at[dest].set(wt[order].astype(hf.dtype))
    blk_start = jnp.arange(n_blk, dtype=jnp.int32) * EXPERT_BLOCK
    blk_e = jnp.minimum(jnp.searchsorted(pad_end, blk_start, side='right'), N_EXPERTS - 1)
    xs = hf[buf_tok].reshape(n_blk, EXPERT_BLOCK, d)

    def run(args):
        xb, e = args
        return (jax.nn.silu(xb @ w_gate[e]) * (xb @ w_up[e])) @ w_down[e]

    ys = lax.map(run, (xs, blk_e)).reshape(p_rows, d)
    return jnp.zeros_like(hf).at[buf_tok].add(ys * buf_w[:, None])


def _hier_moe(h, w_group, b_group, w_expert, b_expert, w_gate, w_up, w_down):
    b, s, d = h.shape
    n = b * s
    hf = h.reshape(n, d)
    g_logits = (hf @ w_group).astype(jnp.float32) + b_group.astype(jnp.float32)
    g_prob = jax.nn.softmax(g_logits, axis=-1)
    grp = jnp.argmax(g_logits, axis=-1).astype(jnp.int32)
    p_grp = jnp.take_along_axis(g_prob, grp[:, None], axis=-1)
    e_logits = ((hf @ w_expert).astype(jnp.float32) + b_expert.astype(jnp.float32)).reshape(
        n, N_GROUPS, EXPERTS_PER_GROUP)
    e_in = jnp.take_along_axis(e_logits, grp[:, None, None], axis=1)[:, 0]
    top_v, top_i = lax.top_k(e_in, TOP_K)
    wts = jax.nn.softmax(top_v, axis=-1) * p_grp
    eid = grp[:, None] * EXPERTS_PER_GROUP + top_i.astype(jnp.int32)
    y = _grouped_experts(hf, eid.reshape(-1), wts.reshape(-1), w_gate, w_up, w_down)
    return y.reshape(b, s, d)


def _layer(x, c, ang_a, ang_row, ang_col, w_ada, b_ada, w_in, a_q_norm, a_kv_norm, a_w_uq, a_w_ukv,
           a_w_o, b_q_norm, b_k_norm, b_w_o, w_out, ln1_g, ln1_b, w_group, b_group, w_expert,
           b_expert, e_w_gate, e_w_up, e_w_down, ln2_g, ln2_b):
    mod = (jax.nn.silu(c) @ w_ada + b_ada)[:, None, :]
    sh1, sc1, g1, sh2, sc2, g2 = jnp.split(mod, 6, axis=-1)
    h = x * (1 + sc1) + sh1
    c_q, c_kv, k_rope, q_b, k_b, v_b, gl_a, gl_b = jnp.split(h @ w_in, IN_OFFSETS, axis=-1)
    y_a = _mla_branch(c_q, c_kv, k_rope, ang_a, a_q_norm, a_kv_norm, a_w_uq, a_w_ukv, a_w_o)
    y_b = _gqa_branch(q_b, k_b, v_b, ang_row, ang_col, b_q_norm, b_k_norm, b_w_o)
    attn = (jax.nn.sigmoid(gl_a) * y_a + jax.nn.sigmoid(gl_b) * y_b) @ w_out
    x = _layer_norm(DEEPNORM_ALPHA * x + g1 * attn, ln1_g, ln1_b)
    h2 = x * (1 + sc2) + sh2
    ffn = _hier_moe(h2, w_group, b_group, w_expert, b_expert, e_w_gate, e_w_up, e_w_down)
    return _layer_norm(DEEPNORM_ALPHA * x + g2 * ffn, ln2_g, ln2_b)


def _trunk(x, c, weights):
    s = x.shape[1]
    rows = s // GRID_W
    t = jnp.arange(s, dtype=jnp.int32)
    row = jnp.repeat(jnp.arange(rows, dtype=jnp.int32), GRID_W)
    col = jnp.tile(jnp.arange(GRID_W, dtype=jnp.int32), rows)
    ang_a = _rope_angles(t, A_ROPE)
    ang_row = _rope_angles(row, B_HEAD_DIM // 2)
    ang_col = _rope_angles(col, B_HEAD_DIM // 2)
    for l in range(DEPTH):
        x = _layer(x, c, ang_a, ang_row, ang_col, *[w[l] for w in weights])
    return x


def setup_inputs(seed: int = 0) -> dict:
    key = jax.random.key(seed)
    ks = jax.random.split(key, 32)
    f32 = jnp.float32
    L = DEPTH
    D = D_MODEL

    def nrm(k, shape, scale):
        return jax.random.normal(k, shape, f32) * scale

    return {
        'x_prompt': nrm(ks[0], (BATCH, SEQ, D), 1.0),
        'x_sample': nrm(ks[1], (DEC_BATCH, DEC_SEQ, D), 1.0),
        'c_prompt': nrm(ks[2], (BATCH, D), 1.0),
        'c_sample': nrm(ks[3], (DEC_BATCH, D), 1.0),
        'w_ada': nrm(ks[4], (L, D, 6 * D), 0.5 * D ** -0.5),
        'b_ada': nrm(ks[5], (L, 6 * D), 0.02),
        'w_in': nrm(ks[6], (L, D, D_IN), D ** -0.5),
        'a_q_norm': 1.0 + nrm(ks[7], (L, A_Q_RANK), 0.02),
        'a_kv_norm': 1.0 + nrm(ks[8], (L, A_KV_RANK), 0.02),
        'a_w_uq': nrm(ks[9], (L, A_Q_RANK, A_HEADS * (A_NOPE + A_ROPE)), A_Q_RANK ** -0.5),
        'a_w_ukv': nrm(ks[10], (L, A_KV_RANK, A_HEADS * (A_NOPE + A_V)), A_KV_RANK ** -0.5),
        'a_w_o': nrm(ks[11], (L, A_HEADS * A_V, D), (A_HEADS * A_V) ** -0.5),
        'b_q_norm': 1.0 + nrm(ks[12], (L, B_HEAD_DIM), 0.02),
        'b_k_norm': 1.0 + nrm(ks[13], (L, B_HEAD_DIM), 0.02),
        'b_w_o': nrm(ks[14], (L, B_HEADS * B_HEAD_DIM, D), (B_HEADS * B_HEAD_DIM) ** -0.5),
        'w_out': nrm(ks[15], (L, D, D), DEEPNORM_BETA * D ** -0.5),
        'ln1_g': 1.0 + nrm(ks[16], (L, D), 0.02),
        'ln1_b': nrm(ks[17], (L, D), 0.02),
        'w_group': nrm(ks[18], (L, D, N_GROUPS), D ** -0.5),
        'b_group': nrm(ks[19], (L, N_GROUPS), 0.01),
        'w_expert': nrm(ks[20], (L, D, N_EXPERTS), D ** -0.5),
        'b_expert': nrm(ks[21], (L, N_EXPERTS), 0.01),
        'e_w_gate': nrm(ks[22], (L, N_EXPERTS, D, D_EXPERT), D ** -0.5),
        'e_w_up': nrm(ks[23], (L, N_EXPERTS, D, D_EXPERT), D ** -0.5),
        'e_w_down': nrm(ks[24], (L, N_EXPERTS, D_EXPERT, D), DEEPNORM_BETA * D_EXPERT ** -0.5),
        'ln2_g': 1.0 + nrm(ks[25], (L, D), 0.02),
        'ln2_b': nrm(ks[26], (L, D), 0.02),
    }


def reference(x_prompt, x_sample, c_prompt, c_sample, w_ada, b_ada, w_in, a_q_norm, a_kv_norm,
              a_w_uq, a_w_ukv, a_w_o, b_q_norm, b_k_norm, b_w_o, w_out, ln1_g, ln1_b, w_group,
              b_group, w_expert, b_expert, e_w_gate, e_w_up, e_w_down, ln2_g, ln2_b):
    weights = (w_ada, b_ada, w_in, a_q_norm, a_kv_norm, a_w_uq, a_w_ukv, a_w_o, b_q_norm, b_k_norm,
               b_w_o, w_out, ln1_g, ln1_b, w_group, b_group, w_expert, b_expert, e_w_gate, e_w_up,
               e_w_down, ln2_g, ln2_b)
    y_prompt = _trunk(x_prompt, c_prompt, weights)
    y_sample = _trunk(x_sample, c_sample, weights)
    return (y_prompt, y_sample)
```

```python
import numpy as np
from contextlib import ExitStack
import concourse.bass as bass
import concourse.mybir as mybir
from concourse.bass_utils import run_bass_kernel_spmd

F32 = mybir.dt.float32
BF16 = mybir.dt.bfloat16
AF = mybir.ActivationFunctionType
ALU = mybir.AluOpType
AX = mybir.AxisListType

D = 2048
KC = 16
NH = 16
NKVH = 4
NE = 64
DE = 512
TQ = 512
ALPHA = 2.0 ** 0.25
RMS_EPS = 1e-6
LN_EPS = 1e-5
THETA = 10000.0
C_CQ, C_CKV, C_KR, C_QB, C_KB, C_VB, C_GA, C_GB = 0, 512, 1024, 1088, 3136, 3648, 4160, 6208
D_IN = 8256


class Tok:
    def __init__(self, sem, name):
        self.sem = sem
        self.name = name
        self.count = 0


class Eng(Tok):
    def __init__(self, eng, sem, name, strict_self):
        super().__init__(sem, name)
        self.eng = eng
        self.waited = {}
        self.strict_self = strict_self


class Buf:
    __slots__ = ("name", "w", "r", "multi")

    def __init__(self, name, multi=False):
        self.name = name
        self.w = {}
        self.r = {}
        self.multi = multi


class Sched:
    def __init__(self, nc, es, n_dma_sems=24):
        self.nc = nc

        def mk(eng, name, strict):
            return Eng(eng, es.enter_context(nc.semaphore("s_" + name)), name, strict)

        self.PE = mk(nc.tensor, "pe", False)
        self.ACT = mk(nc.scalar, "act", True)
        self.DVE = mk(nc.vector, "dve", True)
        self.POOL = mk(nc.gpsimd, "pool", True)
        self.SP = mk(nc.sync, "sp", False)
        self.engs = [self.PE, self.ACT, self.DVE, self.POOL, self.SP]
        self.dma_toks = {}
        for q in ("sp", "pool"):
            self.dma_toks[q] = [Tok(es.enter_context(nc.semaphore(f"d_{q}{i}")), f"d_{q}{i}")
                                for i in range(n_dma_sems)]
        self.dma_rr = {"sp": 0, "pool": 0}
        self.n_inst = 0

    def _waits(self, E, reads, writes):
        need = {}
        for b in reads:
            for t, v in b.w.items():
                if need.get(t, 0) < v:
                    need[t] = v
        for b in writes:
            if b.multi:
                continue
            for t, v in b.w.items():
                if need.get(t, 0) < v:
                    need[t] = v
            for t, v in b.r.items():
                if need.get(t, 0) < v:
                    need[t] = v
        for t, v in need.items():
            if t is E and not E.strict_self:
                continue
            if E.waited.get(t, 0) >= v:
                continue
            E.eng.wait_ge(t.sem, v)
            E.waited[t] = v
            self.n_inst += 1

    def _mark(self, tok, mark, reads, writes):
        for b in reads:
            if b.r.get(tok, 0) < mark:
                b.r[tok] = mark
        for b in writes:
            if b.multi:
                if b.w.get(tok, 0) < mark:
                    b.w[tok] = mark
            else:
                b.w = {tok: mark}
                b.r = {}

    def op(self, E, fn, reads=(), writes=(), signal=True):
        self._waits(E, reads, writes)
        ins = fn()
        self.n_inst += 1
        if signal:
            E.count += 1
            ins.then_inc(E.sem, 1)
            mark = E.count
        else:
            mark = E.count + 1
        self._mark(E, mark, reads, writes)
        return ins

    def dma(self, out_ap, in_ap, reads=(), writes=(), q="sp"):
        E = self.SP if q == "sp" else self.POOL
        toks = self.dma_toks[q]
        t = toks[self.dma_rr[q] % len(toks)]
        self.dma_rr[q] += 1
        if t.count > 0 and E.waited.get(t, 0) < t.count:
            E.eng.wait_ge(t.sem, t.count)
            E.waited[t] = t.count
        self._waits(E, reads, writes)
        ins = E.eng.dma_start(out=out_ap, in_=in_ap)
        t.count += 16
        ins.then_inc(t.sem, 16)
        self.n_inst += 1
        self._mark(t, t.count, reads, writes)
        return ins

    def barrier(self):
        toks = list(self.engs)
        for q in self.dma_toks.values():
            toks += q
        for E in self.engs:
            for t in toks:
                if t is E or t.count == 0 or E.waited.get(t, 0) >= t.count:
                    continue
                E.eng.wait_ge(t.sem, t.count)
                E.waited[t] = t.count


def build(NKP, NQP, NKS, NQS):
    T = NQP + NQS
    nc = bass.Bass("TRN2", target_bir_lowering=False)

    def din(name, shape, dt=F32):
        return nc.dram_tensor(name, list(shape), dt, kind="ExternalInput").ap()

    def dscr(name, shape, dt):
        return nc.dram_tensor(name, list(shape), dt).ap()

    NK = [NKP, NKS]
    NQ = [NQP, NQS]
    xk = [din("xkp", [D, NKP]), din("xks", [D, NKS])]
    xq = [din("xqp", [D, NQP]), din("xqs", [D, NQS])]
    tAk = [din("tAkp", [2, 64, NKP]), din("tAks", [2, 64, NKS])]
    tBk = [din("tBkp", [2, 128, NKP]), din("tBks", [2, 128, NKS])]
    tAq = [din("tAqp", [2, 64, NQP]), din("tAqs", [2, 64, NQS])]
    tBq = [din("tBqp", [2, 128, NQP]), din("tBqs", [2, 128, NQS])]
    cT_d = din("cT", [128, KC, 2])
    RA_d = din("RA", [64, 64])
    RB_d = din("RB", [128, 128])
    ident_d = din("ident", [128, 128])
    w_ada_d = din("w_ada", [D, 6 * D])
    b_ada_d = din("b_adaT", [128, 96])
    w_in_d = din("w_in", [D, D_IN])
    aqn_d = din("a_q_norm", [128, 4])
    akvn_d = din("a_kv_norm", [128, 4])
    w_uq_d = din("a_w_uq", [512, 3072])
    w_ukv_d = din("a_w_ukv", [512, 4096])
    w_ao_d = din("a_w_o", [D, D])
    bqn_d = din("b_q_norm", [128, 1])
    bkn_d = din("b_k_norm", [128, 1])
    w_bo_d = din("b_w_o", [D, D])
    w_out_d = din("w_out", [D, D])
    ln_d = din("ln_gb", [128, 4, KC])
    w_r_d = din("w_r", [D, 72])
    b_r_d = din("b_r", [128, 72])
    eg_d = din("e_w_gate", [NE, D, DE])
    eu_d = din("e_w_up", [NE, D, DE])
    ed_d = din("e_w_down", [NE, DE, D])
    y_d = [nc.dram_tensor("yp", [D, NQP], F32, kind="ExternalOutput").ap(),
           nc.dram_tensor("ys", [D, NQS], F32, kind="ExternalOutput").ap()]

    w_in_b = dscr("w_in_b", [D, D_IN], BF16)
    w_uq_b = dscr("w_uq_b", [512, 3072], BF16)
    w_ukv_b = dscr("w_ukv_b", [512, 4096], BF16)
    w_ao_b = dscr("w_ao_b", [D, D], BF16)
    w_bo_b = dscr("w_bo_b", [D, D], BF16)
    w_out_b = dscr("w_out_b", [D, D], BF16)
    eg_b = dscr("eg_b", [NE, D, DE], BF16)
    eu_b = dscr("eu_b", [NE, D, DE], BF16)
    ed_b = dscr("ed_b", [NE, DE, D], BF16)
    KA = [dscr(f"KA{j}", [NH, 128, NK[j]], BF16) for j in range(2)]
    KR = [dscr(f"KR{j}", [64, NK[j]], BF16) for j in range(2)]
    VA = [dscr(f"VA{j}", [NH, NK[j], 128], BF16) for j in range(2)]
    KB = [dscr(f"KB{j}", [NKVH, 128, NK[j]], BF16) for j in range(2)]
    VB = [dscr(f"VB{j}", [NKVH, NK[j], 128], BF16) for j in range(2)]
    x1n_s = dscr("x1n_s", [D, T], F32)
    h2_s = dscr("h2_s", [D, T], BF16)
    cw_s = dscr("cw_s", [NE, T], F32)

    with ExitStack() as es:
        S = Sched(nc, es)
        PE, ACT, DVE, POOL = S.PE, S.ACT, S.DVE, S.POOL

        def sb(stack, name, shape, dt):
            return stack.enter_context(nc.sbuf_tensor("sb_" + name, list(shape), dt)), Buf(name)

        ps = [es.enter_context(nc.psum_tensor(f"ps{i}", [128, 512], F32)) for i in range(8)]
        bps = [Buf(f"ps{i}") for i in range(8)]
        gp_state = [0]

        def gp():
            i = gp_state[0] % 4
            gp_state[0] += 1
            return i

        ones_bf, b_ones = sb(es, "ones_bf", [128, 128], BF16)
        onesD_bf, b_onesD = sb(es, "onesD_bf", [128, 128], BF16)
        RA_bf, b_RA = sb(es, "RA_bf", [64, 64], BF16)
        RB_bf, b_RB = sb(es, "RB_bf", [128, 128], BF16)
        ident, b_ident = sb(es, "ident", [128, 128], F32)
        epsr, b_epsr = sb(es, "epsr", [128, 1], F32)
        epsl, b_epsl = sb(es, "epsl", [128, 1], F32)
        mod, b_mod = sb(es, "mod", [128, 96, 2], F32)
        aqn, b_aqn = sb(es, "aqn", [128, 4], F32)
        akvn, b_akvn = sb(es, "akvn", [128, 4], F32)
        bqn, b_bqn = sb(es, "bqn", [128, 1], F32)
        bkn, b_bkn = sb(es, "bkn", [128, 1], F32)
        lngb, b_lngb = sb(es, "lngb", [128, 4, KC], F32)
        w_r, b_w_r = sb(es, "w_r", [128, KC, 72], F32)
        b_r, b_b_r = sb(es, "b_r", [128, 72], F32)
        stage, b_stage = sb(es, "stage", [128, 128], F32)

        S.op(DVE, lambda: nc.vector.memset(ones_bf[:], 1.0), writes=[b_ones])
        S.op(DVE, lambda: nc.vector.memset(onesD_bf[:], 1.0 / D), writes=[b_onesD])
        S.op(DVE, lambda: nc.vector.memset(epsr[:], RMS_EPS), writes=[b_epsr])
        S.op(DVE, lambda: nc.vector.memset(epsl[:], LN_EPS), writes=[b_epsl])
        S.dma(stage[:], RB_d, writes=[b_stage])
        S.op(DVE, lambda: nc.vector.tensor_copy(RB_bf[:], stage[:]), reads=[b_stage], writes=[b_RB])
        S.dma(stage[0:64, 0:64], RA_d, writes=[b_stage])
        S.op(DVE, lambda: nc.vector.tensor_copy(RA_bf[:], stage[0:64, 0:64]), reads=[b_stage], writes=[b_RA])
        S.dma(ident[:], ident_d, writes=[b_ident])
        S.dma(aqn[:], aqn_d, writes=[b_aqn])
        S.dma(akvn[:], akvn_d, writes=[b_akvn])
        S.dma(bqn[:], bqn_d, writes=[b_bqn])
        S.dma(bkn[:], bkn_d, writes=[b_bkn])
        S.dma(lngb[:], ln_d, writes=[b_lngb])
        S.dma(w_r[:], w_r_d.rearrange("(kc p) n -> p kc n", p=128), writes=[b_w_r])
        S.dma(b_r[:], b_r_d, writes=[b_b_r])

        bw = {k: Buf(k, multi=True) for k in ("w_in_b", "w_uq_b", "w_ukv_b", "w_ao_b", "w_bo_b", "w_out_b", "e_b")}
        R4 = D // 4
        for i in range(4):
            S.dma(w_in_b[i * R4:(i + 1) * R4, :], w_in_d[i * R4:(i + 1) * R4, :], writes=[bw["w_in_b"]], q="pool")
        S.dma(w_uq_b, w_uq_d, writes=[bw["w_uq_b"]], q="pool")
        S.dma(w_ukv_b, w_ukv_d, writes=[bw["w_ukv_b"]], q="pool")
        S.dma(w_ao_b, w_ao_d, writes=[bw["w_ao_b"]], q="pool")
        S.dma(w_bo_b, w_bo_d, writes=[bw["w_bo_b"]], q="pool")
        S.dma(w_out_b, w_out_d, writes=[bw["w_out_b"]], q="pool")

        with ExitStack() as es0:
            cs, b_cs = sb(es0, "cs", [128, KC, 2], F32)
            badaT, b_badaT = sb(es0, "badaT", [128, 96], F32)
            wa = [sb(es0, f"wa{i}", [128, KC, 128], F32) for i in range(3)]
            S.dma(cs[:], cT_d, writes=[b_cs])
            S.dma(badaT[:], b_ada_d, writes=[b_badaT])
            S.op(ACT, lambda: nc.scalar.activation(out=cs[:], in_=cs[:], func=AF.Silu), reads=[b_cs], writes=[b_cs])
            wav = w_ada_d.rearrange("(kc p) n -> p kc n", p=128)
            for j in range(96 + 2):
                if j < 96:
                    t_, b_ = wa[j % 3]
                    S.dma(t_[:], wav[:, :, j * 128:(j + 1) * 128], writes=[b_])
                jj = j - 2
                if jj >= 0:
                    t_, b_ = wa[jj % 3]
                    pi = gp()
                    for kc in range(KC):
                        S.op(PE, lambda t_=t_, kc=kc, pi=pi: nc.tensor.matmul(
                            ps[pi][:, 0:2], t_[:, kc, :], cs[:, kc, :], start=(kc == 0), stop=(kc == KC - 1)),
                            reads=[b_, b_cs], writes=[bps[pi]], signal=(kc == KC - 1))
                    S.op(DVE, lambda jj=jj, pi=pi: nc.vector.tensor_scalar(
                        out=mod[:, jj, :], in0=ps[pi][:, 0:2], scalar1=badaT[:, jj:jj + 1], scalar2=None, op0=ALU.add),
                        reads=[bps[pi], b_badaT], writes=[b_mod])
            for lo in (16, 64):
                S.op(DVE, lambda lo=lo: nc.vector.tensor_scalar(
                    out=mod[:, lo:lo + 16, :], in0=mod[:, lo:lo + 16, :], scalar1=1.0, scalar2=None, op0=ALU.add),
                    reads=[b_mod], writes=[b_mod])
        S.barrier()

        cast_list = []
        for e in range(NE):
            cast_list += [(eg_b[e], eg_d[e]), (eu_b[e], eu_d[e]), (ed_b[e], ed_d[e])]
        n_a_tiles = (NKP + NKS) // TQ
        casts_per_tile = -(-len(cast_list) // n_a_tiles)

        def emit_casts(n):
            for _ in range(n):
                if cast_list:
                    d_, s_ = cast_list.pop(0)
                    S.dma(d_, s_, writes=[bw["e_b"]], q="pool")

        SH1, SC1, G1, SH2, SC2, G2 = 0, 16, 32, 48, 64, 80
        bKV = [Buf("kv0", multi=True), Buf("kv1", multi=True)]
        b_x1n_s, b_h2_s, b_cw_s = Buf("x1n_s", multi=True), Buf("h2_s", multi=True), Buf("cw_s", multi=True)

        with ExitStack() as es1:
            xT, b_xT = sb(es1, "xT", [128, KC, TQ], F32)
            hT, b_hT = sb(es1, "hT", [128, KC, TQ], BF16)
            qAn, b_qAn = sb(es1, "qAn", [128, NH, TQ], BF16)
            qAr, b_qAr = sb(es1, "qAr", [128, NH, TQ], BF16)
            qB, b_qB = sb(es1, "qB", [128, NH, TQ], BF16)
            bq_heads = {"qAn": [Buf(f"qAn{h}") for h in range(NH)], "qAr": [Buf(f"qAr{h}") for h in range(NH)],
                        "qB": [Buf(f"qB{h}") for h in range(NH)]}
            NWS = 3
            wslot = [sb(es1, f"wslot{i}", [128, KC, 256], BF16) for i in range(NWS)]
            ws_state = [0]
            KCH = 1024
            NKS_ = 3
            kts = [sb(es1, f"kt{i}", [128, KCH], BF16) for i in range(NKS_)]
            krs = [sb(es1, f"kr{i}", [64, KCH], BF16) for i in range(NKS_)]
            vvs = [sb(es1, f"vv{i}", [128, KCH // 128, 128], BF16) for i in range(NKS_)]
            pTs = [sb(es1, f"pT{i}", [128, 512], BF16) for i in range(4)]
            craw, b_craw = sb(es1, "craw", [128, 4, TQ], F32)
            cn, b_cn = sb(es1, "cn", [128, 4, TQ], BF16)
            tcA, b_tcA = sb(es1, "tcA", [64, 2, TQ], F32)
            tcB, b_tcB = sb(es1, "tcB", [128, 2, TQ], F32)
            rstd, b_rstd = sb(es1, "rstd", [128, TQ], F32)
            mean, b_mean = sb(es1, "mean", [128, TQ], F32)
            tmp32 = [sb(es1, f"tmp32_{i}", [128, TQ], F32) for i in range(5)]
            tmp16 = [sb(es1, f"tmp16_{i}", [128, TQ], BF16) for i in range(4)]
            t32_state = [0]
            t16_state = [0]
            lg, b_lg = sb(es1, "lg", [128, 72], F32)
            sm = [sb(es1, f"sm{i}", [128, 8], F32) for i in range(8)]
            sc = [sb(es1, f"sc{i}", [128, 1], F32) for i in range(10)]
            cw, b_cw = sb(es1, "cw", [128, NE], F32)
            cwT, b_cwT = sb(es1, "cwT", [64, TQ], F32)

            def t32():
                i = t32_state[0] % len(tmp32)
                t32_state[0] += 1
                return tmp32[i]

            def t16():
                i = t16_state[0] % len(tmp16)
                t16_state[0] += 1
                return tmp16[i]

            def load_w(src, bsrc, krows, c0, ncols):
                i = ws_state[0] % NWS
                ws_state[0] += 1
                t_, b_ = wslot[i]
                kcn = krows // 128
                S.dma(t_[:, 0:kcn, 0:ncols], src.rearrange("(kc p) n -> p kc n", p=128)[:, :, c0:c0 + ncols],
                      reads=[bsrc], writes=[b_])
                return t_, b_

            def run_blocks(specs, pre=2):
                loaded = []
                for i in range(len(specs) + pre):
                    if i < len(specs):
                        sp_ = specs[i]
                        loaded.append(load_w(sp_[0], sp_[1], sp_[2], sp_[3], sp_[4]))
                    j = i - pre
                    if j >= 0:
                        specs[j][5](*loaded[j])

            def proj(pi, wt, bwt, kcn, c0, m, rhs_fn, rhs_bufs, prow=128):
                for kc in range(kcn):
                    S.op(PE, lambda kc=kc: nc.tensor.matmul(
                        ps[pi][0:m, :], wt[0:prow, kc, c0:c0 + m], rhs_fn(kc), start=(kc == 0), stop=(kc == kcn - 1)),
                        reads=[bwt] + rhs_bufs, writes=[bps[pi]], signal=(kc == kcn - 1))

            def load_x_tile(src, t0, job):
                S.dma(xT[:], src.rearrange("(kc p) n -> p kc n", p=128)[:, :, t0:t0 + TQ], writes=[b_xT])
                for kc in range(KC):
                    S.op(DVE, lambda kc=kc: nc.vector.tensor_scalar(
                        out=hT[:, kc, :], in0=xT[:, kc, :], scalar1=mod[:, SC1 + kc, job:job + 1],
                        scalar2=mod[:, SH1 + kc, job:job + 1], op0=ALU.mult, op1=ALU.add),
                        reads=[b_xT, b_mod], writes=[b_hT])

            def load_tables(tA, tB, t0):
                S.dma(tcA[:], tA.rearrange("c p n -> p c n")[:, :, t0:t0 + TQ], writes=[b_tcA])
                S.dma(tcB[:], tB.rearrange("c p n -> p c n")[:, :, t0:t0 + TQ], writes=[b_tcB])

            def rms_lowrank(specs_cols, gain, c_off):
                def consume(blk):
                    def f(wt, bwt):
                        for cc in range(2):
                            c = blk * 2 + cc
                            pi = gp()
                            proj(pi, wt, bwt, KC, cc * 128, 128, lambda kc: hT[:, kc, :], [b_hT])
                            S.op(ACT, lambda: nc.scalar.copy(out=craw[:, c, :], in_=ps[pi][:]),
                                 reads=[bps[pi]], writes=[b_craw])
                            sq, bsq = t16()
                            S.op(ACT, lambda: nc.scalar.activation(out=sq[:], in_=ps[pi][:], func=AF.Square),
                                 reads=[bps[pi]], writes=[bsq])
                            S.op(PE, lambda: nc.tensor.matmul(ps[6][:], ones_bf[:], sq[:], start=(c == 0), stop=(c == 3)),
                                 reads=[b_ones, bsq], writes=[bps[6]], signal=True)
                    return f
                run_blocks([(w_in_b, bw["w_in_b"], D, c_off + blk * 256, 256, consume(blk)) for blk in range(2)])
                S.op(ACT, lambda: nc.scalar.activation(out=rstd[:], in_=ps[6][:], func=AF.Sqrt, bias=epsr[:], scale=1.0 / 512),
                     reads=[bps[6], b_epsr], writes=[b_rstd])
                S.op(DVE, lambda: nc.vector.reciprocal(out=rstd[:], in_=rstd[:]), reads=[b_rstd], writes=[b_rstd])
                for c in range(4):
                    S.op(DVE, lambda c=c: nc.vector.scalar_tensor_tensor(
                        out=cn[:, c, :], in0=craw[:, c, :], scalar=gain[0][:, c:c + 1], in1=rstd[:], op0=ALU.mult, op1=ALU.mult),
                        reads=[b_craw, gain[1], b_rstd], writes=[b_cn])

            def rope_finish(pi_raw, raw16, braw16, R_bf, bR, tab, btab, rows, out_ap, out_bufs):
                pj = gp()
                S.op(PE, lambda: nc.tensor.matmul(ps[pj][0:rows, :], R_bf[:], raw16[0:rows, :], start=True, stop=True),
                     reads=[bR, braw16], writes=[bps[pj]])
                ta, bta = t32()
                tb, btb = t32()
                S.op(DVE, lambda: nc.vector.tensor_tensor(out=ta[0:rows, :], in0=raw16[0:rows, :], in1=tab[0:rows, 0, :], op=ALU.mult),
                     reads=[braw16, btab], writes=[bta])
                S.op(DVE, lambda: nc.vector.tensor_tensor(out=tb[0:rows, :], in0=ps[pj][0:rows, :], in1=tab[0:rows, 1, :], op=ALU.mult),
                     reads=[bps[pj], btab], writes=[btb])
                S.op(DVE, lambda: nc.vector.tensor_tensor(out=out_ap, in0=ta[0:rows, :], in1=tb[0:rows, :], op=ALU.add),
                     reads=[bta, btb], writes=out_bufs)

            def headnorm_rope(pi, gainb, out_ap, out_bufs):
                sq, bsq = t16()
                S.op(ACT, lambda: nc.scalar.activation(out=sq[:], in_=ps[pi][:], func=AF.Square), reads=[bps[pi]], writes=[bsq])
                pj = gp()
                S.op(PE, lambda: nc.tensor.matmul(ps[pj][:], ones_bf[:], sq[:], start=True, stop=True),
                     reads=[b_ones, bsq], writes=[bps[pj]])
                rs, brs = t32()
                S.op(ACT, lambda: nc.scalar.activation(out=rs[:], in_=ps[pj][:], func=AF.Sqrt, bias=epsr[:], scale=1.0 / 128),
                     reads=[bps[pj], b_epsr], writes=[brs])
                S.op(DVE, lambda: nc.vector.reciprocal(out=rs[:], in_=rs[:]), reads=[brs], writes=[brs])
                kn, bkn_ = t16()
                S.op(DVE, lambda: nc.vector.scalar_tensor_tensor(
                    out=kn[:], in0=ps[pi][:], scalar=gainb[0][:, 0:1], in1=rs[:], op0=ALU.mult, op1=ALU.mult),
                    reads=[bps[pi], gainb[1], brs], writes=[bkn_])
                rope_finish(pi, kn, bkn_, RB_bf, b_RB, tcB, b_tcB, 128, out_ap, out_bufs)

            def phase_a(job):
                nk = NK[job]
                for t in range(nk // TQ):
                    t0 = t * TQ
                    emit_casts(casts_per_tile)
                    load_x_tile(xk[job], t0, job)
                    load_tables(tAk[job], tBk[job], t0)
                    rms_lowrank(None, (akvn, b_akvn), C_CKV)

                    specs = []

                    def kr_consume(wt, bwt):
                        pi = gp()
                        proj(pi, wt, bwt, KC, 0, 64, lambda kc: hT[:, kc, :], [b_hT])
                        r16, br16 = t16()
                        S.op(ACT, lambda: nc.scalar.copy(out=r16[0:64, :], in_=ps[pi][0:64, :]), reads=[bps[pi]], writes=[br16])
                        o16, bo16 = t16()
                        rope_finish(pi, r16, br16, RA_bf, b_RA, tcA, b_tcA, 64, o16[0:64, :], [bo16])
                        S.dma(KR[job][:, t0:t0 + TQ], o16[0:64, :], reads=[bo16], writes=[bKV[job]])
                    specs.append((w_in_b, bw["w_in_b"], D, C_KR, 64, kr_consume))

                    def kb_consume(blk):
                        def f(wt, bwt):
                            for cc in range(2):
                                kvh = blk * 2 + cc
                                pi = gp()
                                proj(pi, wt, bwt, KC, cc * 128, 128, lambda kc: hT[:, kc, :], [b_hT])
                                o16, bo16 = t16()
                                headnorm_rope(pi, (bkn, b_bkn), o16[:], [bo16])
                                S.dma(KB[job][kvh, :, t0:t0 + TQ], o16[:], reads=[bo16], writes=[bKV[job]])
                        return f
                    for blk in range(2):
                        specs.append((w_in_b, bw["w_in_b"], D, C_KB + blk * 256, 256, kb_consume(blk)))

                    def vb_consume(blk):
                        def f(wt, bwt):
                            for half in range(2):
                                pi = gp()
                                for s2 in range(2):
                                    sub = half * 2 + s2
                                    for kc in range(KC):
                                        S.op(PE, lambda kc=kc, sub=sub, s2=s2: nc.tensor.matmul(
                                            ps[pi][:, s2 * 256:(s2 + 1) * 256], hT[:, kc, sub * 128:(sub + 1) * 128],
                                            wt[:, kc, 0:256], start=(kc == 0), stop=(kc == KC - 1)),
                                            reads=[bwt, b_hT], writes=[bps[pi]], signal=(kc == KC - 1 and s2 == 1))
                                o16, bo16 = t16()
                                S.op(ACT, lambda: nc.scalar.copy(out=o16[:], in_=ps[pi][:]), reads=[bps[pi]], writes=[bo16])
                                for s2 in range(2):
                                    sub = half * 2 + s2
                                    for hh in range(2):
                                        kvh = blk * 2 + hh
                                        S.dma(VB[job][kvh, t0 + sub * 128:t0 + (sub + 1) * 128, :],
                                              o16[:, s2 * 256 + hh * 128:s2 * 256 + (hh + 1) * 128], reads=[bo16], writes=[bKV[job]])
                        return f
                    for blk in range(2):
                        specs.append((w_in_b, bw["w_in_b"], D, C_VB + blk * 256, 256, vb_consume(blk)))

                    def ukv_consume(h):
                        def f(wt, bwt):
                            pi = gp()
                            proj(pi, wt, bwt, 4, 0, 128, lambda kc: cn[:, kc, :], [b_cn])
                            o16, bo16 = t16()
                            S.op(ACT, lambda: nc.scalar.copy(out=o16[:], in_=ps[pi][:]), reads=[bps[pi]], writes=[bo16])
                            S.dma(KA[job][h, :, t0:t0 + TQ], o16[:], reads=[bo16], writes=[bKV[job]])
                            pj = gp()
                            for sub in range(4):
                                for kc in range(4):
                                    S.op(PE, lambda kc=kc, sub=sub: nc.tensor.matmul(
                                        ps[pj][:, sub * 128:(sub + 1) * 128], cn[:, kc, sub * 128:(sub + 1) * 128],
                                        wt[:, kc, 128:256], start=(kc == 0), stop=(kc == 3)),
                                        reads=[bwt, b_cn], writes=[bps[pj]], signal=(kc == 3 and sub == 3))
                            v16, bv16 = t16()
                            S.op(DVE, lambda: nc.vector.tensor_copy(v16[:], ps[pj][:]), reads=[bps[pj]], writes=[bv16])
                            S.dma(VA[job][h, t0:t0 + TQ, :].rearrange("(s p) d -> p s d", p=128),
                                  v16[:].rearrange("p (s d) -> p s d", d=128), reads=[bv16], writes=[bKV[job]])
                        return f
                    for h in range(NH):
                        specs.append((w_ukv_b, bw["w_ukv_b"], 512, h * 256, 256, ukv_consume(h)))
                    run_blocks(specs)

            def attn_head(job, kparts, vsrc, q_aps, q_bufs, out_ap, out_bufs, scale, hidx):
                nk = NK[job]
                nch = nk // KCH
                po = 4 + (hidx % 2)
                pz = 6 + (hidx % 2)
                nblk = KCH // 128
                chunk_bufs = {}

                def load_chunk(c):
                    i = attn_state[0] % NKS_
                    attn_state[0] += 1
                    got = []
                    for (ksrc, rows, slots) in kparts:
                        t_, b_ = slots[i]
                        S.dma(t_[0:rows, :], ksrc[:, c * KCH:(c + 1) * KCH], reads=[bKV[job]], writes=[b_])
                        got.append((t_, b_, rows))
                    vt, bvt = vvs[i]
                    S.dma(vt[:], vsrc[c * KCH:(c + 1) * KCH, :].rearrange("(b p) d -> p b d", p=128), reads=[bKV[job]], writes=[bvt])
                    chunk_bufs[c] = (got, vt, bvt)

                items = [(c, kb) for c in range(nch) for kb in range(nblk)]
                n = len(items)
                sbank = {}

                def emit_s(i):
                    c, kb = items[i]
                    got, vt, bvt = chunk_bufs[c]
                    pi = gp()
                    sbank[i] = pi
                    np_ = len(got)
                    for j, (t_, b_, rows) in enumerate(got):
                        S.op(PE, lambda t_=t_, rows=rows, j=j: nc.tensor.matmul(
                            ps[pi][:], t_[0:rows, kb * 128:(kb + 1) * 128], q_aps[j], start=(j == 0), stop=(j == np_ - 1)),
                            reads=[b_] + q_bufs, writes=[bps[pi]], signal=(j == np_ - 1))

                load_chunk(0)
                if nch > 1:
                    load_chunk(1)
                LA = 2
                for i in range(min(LA, n)):
                    emit_s(i)
                for i in range(n):
                    c, kb = items[i]
                    if kb == 0 and c + 2 < nch:
                        load_chunk(c + 2)
                    if i + LA < n:
                        emit_s(i + LA)
                    pi = sbank.pop(i)
                    pt, bpt = pTs[i % 4]
                    S.op(ACT, lambda pt=pt, pi=pi: nc.scalar.activation(out=pt[:], in_=ps[pi][:], func=AF.Exp, scale=scale),
                         reads=[bps[pi]], writes=[bpt])
                    got, vt, bvt = chunk_bufs[c]
                    S.op(PE, lambda vt=vt, pt=pt, kb=kb: nc.tensor.matmul(
                        ps[po][:], vt[:, kb, :], pt[:], start=(i == 0), stop=(i == n - 1)),
                        reads=[bvt, bpt], writes=[bps[po]], signal=False)
                    S.op(PE, lambda pt=pt: nc.tensor.matmul(
                        ps[pz][:], ones_bf[:], pt[:], start=(i == 0), stop=(i == n - 1)),
                        reads=[b_ones, bpt], writes=[bps[pz]], signal=True)
                rs, brs = t32()
                S.op(DVE, lambda: nc.vector.reciprocal(out=rs[:], in_=ps[pz][:]), reads=[bps[pz]], writes=[brs])
                S.op(DVE, lambda: nc.vector.tensor_tensor(out=out_ap, in0=ps[po][:], in1=rs[:], op=ALU.mult),
                     reads=[bps[po], brs], writes=out_bufs)

            attn_state = [0]

            def layer_norm_inplace(X, bX, g_idx, b_idx):
                for dc in range(KC):
                    xb, bxb = t16()
                    S.op(ACT, lambda dc=dc, xb=xb: nc.scalar.copy(out=xb[:], in_=X[:, dc, :]), reads=[bX], writes=[bxb])
                    sq, bsq = t16()
                    S.op(ACT, lambda dc=dc, sq=sq: nc.scalar.activation(out=sq[:], in_=X[:, dc, :], func=AF.Square), reads=[bX], writes=[bsq])
                    S.op(PE, lambda dc=dc, xb=xb: nc.tensor.matmul(ps[4][:], onesD_bf[:], xb[:], start=(dc == 0), stop=(dc == KC - 1)),
                         reads=[b_onesD, bxb], writes=[bps[4]], signal=True)
                    S.op(PE, lambda dc=dc, sq=sq: nc.tensor.matmul(ps[5][:], onesD_bf[:], sq[:], start=(dc == 0), stop=(dc == KC - 1)),
                         reads=[b_onesD, bsq], writes=[bps[5]], signal=True)
                S.op(ACT, lambda: nc.scalar.copy(out=mean[:], in_=ps[4][:]), reads=[bps[4]], writes=[b_mean])
                m2, bm2 = t32()
                S.op(DVE, lambda: nc.vector.tensor_tensor(out=m2[:], in0=mean[:], in1=mean[:], op=ALU.mult), reads=[b_mean], writes=[bm2])
                S.op(DVE, lambda: nc.vector.tensor_tensor(out=rstd[:], in0=ps[5][:], in1=m2[:], op=ALU.subtract), reads=[bps[5], bm2], writes=[b_rstd])
                S.op(ACT, lambda: nc.scalar.activation(out=rstd[:], in_=rstd[:], func=AF.Sqrt, bias=epsl[:], scale=1.0),
                     reads=[b_rstd, b_epsl], writes=[b_rstd])
                S.op(DVE, lambda: nc.vector.reciprocal(out=rstd[:], in_=rstd[:]), reads=[b_rstd], writes=[b_rstd])
                for dc in range(KC):
                    S.op(POOL, lambda dc=dc: nc.gpsimd.tensor_tensor(out=X[:, dc, :], in0=X[:, dc, :], in1=mean[:], op=ALU.subtract),
                         reads=[bX, b_mean], writes=[bX])
                    S.op(DVE, lambda dc=dc: nc.vector.tensor_tensor(out=X[:, dc, :], in0=X[:, dc, :], in1=rstd[:], op=ALU.mult),
                         reads=[bX, b_rstd], writes=[bX])
                    S.op(DVE, lambda dc=dc: nc.vector.tensor_scalar(
                        out=X[:, dc, :], in0=X[:, dc, :], scalar1=lngb[:, g_idx, dc:dc + 1], scalar2=lngb[:, b_idx, dc:dc + 1],
                        op0=ALU.mult, op1=ALU.add), reads=[bX, b_lngb], writes=[bX])

            def phase_b(job, t, tok0):
                t0 = t * TQ
                load_x_tile(xq[job], t0, job)
                load_tables(tAq[job], tBq[job], t0)
                S.op(ACT, lambda: nc.scalar.mul(out=xT[:], in_=xT[:], mul=ALPHA), reads=[b_xT, b_hT], writes=[b_xT])
                rms_lowrank(None, (aqn, b_aqn), C_CQ)

                specs = []

                def uq_consume(h):
                    def f(wt, bwt):
                        pi = gp()
                        proj(pi, wt, bwt, 4, 0, 128, lambda kc: cn[:, kc, :], [b_cn])
                        S.op(ACT, lambda: nc.scalar.copy(out=qAn[:, h, :], in_=ps[pi][:]), reads=[bps[pi]], writes=[bq_heads["qAn"][h]])
                        pj = gp()
                        proj(pj, wt, bwt, 4, 128, 64, lambda kc: cn[:, kc, :], [b_cn])
                        r16, br16 = t16()
                        S.op(ACT, lambda: nc.scalar.copy(out=r16[0:64, :], in_=ps[pj][0:64, :]), reads=[bps[pj]], writes=[br16])
                        rope_finish(pj, r16, br16, RA_bf, b_RA, tcA, b_tcA, 64, qAr[0:64, h, :], [bq_heads["qAr"][h]])
                    return f
                for h in range(NH):
                    specs.append((w_uq_b, bw["w_uq_b"], 512, h * 192, 192, uq_consume(h)))

                def qb_consume(blk):
                    def f(wt, bwt):
                        for cc in range(2):
                            h = blk * 2 + cc
                            pi = gp()
                            proj(pi, wt, bwt, KC, cc * 128, 128, lambda kc: hT[:, kc, :], [b_hT])
                            headnorm_rope(pi, (bqn, b_bqn), qB[:, h, :], [bq_heads["qB"][h]])
                    return f
                for blk in range(8):
                    specs.append((w_in_b, bw["w_in_b"], D, C_QB + blk * 256, 256, qb_consume(blk)))
                run_blocks(specs)

                for h in range(NH):
                    attn_head(job, [(KA[job][h], 128, kts), (KR[job], 64, krs)], VA[job][h],
                              [qAn[:, h, :], qAr[0:64, h, :]], [bq_heads["qAn"][h], bq_heads["qAr"][h]],
                              qAn[:, h, :], [bq_heads["qAn"][h]], 192.0 ** -0.5, h)
                for h in range(NH):
                    kvh = h // 4
                    attn_head(job, [(KB[job][kvh], 128, kts)], VB[job][kvh],
                              [qB[:, h, :]], [bq_heads["qB"][h]], qB[:, h, :], [bq_heads["qB"][h]], 128.0 ** -0.5, h)

                mT, b_mT = qAr, b_qAr
                allA = bq_heads["qAn"] + bq_heads["qAr"]
                specs = []
                ta_keep = {}

                def o_consume(which, dcp):
                    src, bsrc_heads, base = (qAn, bq_heads["qAn"], 4) if which == 0 else (qB, bq_heads["qB"], 6)

                    def f(wt, bwt):
                        for cc in range(2):
                            proj(base + cc, wt, bwt, KC, cc * 128, 128, lambda kc: src[:, kc, :], bsrc_heads)
                    return f

                def g_consume(which, dcp):
                    base = 4 if which == 0 else 6

                    def f(wt, bwt):
                        for cc in range(2):
                            dc = dcp * 2 + cc
                            pi = gp()
                            proj(pi, wt, bwt, KC, cc * 128, 128, lambda kc: hT[:, kc, :], [b_hT])
                            sg, bsg = t32()
                            S.op(ACT, lambda: nc.scalar.activation(out=sg[:], in_=ps[pi][:], func=AF.Sigmoid), reads=[bps[pi]], writes=[bsg])
                            if which == 0:
                                ta, bta = t32()
                                S.op(DVE, lambda: nc.vector.tensor_tensor(out=ta[:], in0=ps[base + cc][:], in1=sg[:], op=ALU.mult),
                                     reads=[bps[base + cc], bsg], writes=[bta])
                                ta_keep[cc] = (ta, bta)
                            else:
                                ta, bta = ta_keep[cc]
                                tb, btb = t32()
                                S.op(DVE, lambda: nc.vector.tensor_tensor(out=tb[:], in0=ps[base + cc][:], in1=sg[:], op=ALU.mult),
                                     reads=[bps[base + cc], bsg], writes=[btb])
                                S.op(DVE, lambda: nc.vector.tensor_tensor(out=mT[:, dc, :], in0=ta[:], in1=tb[:], op=ALU.add),
                                     reads=[bta, btb], writes=[b_mT] + bq_heads["qAr"])
                    return f
                for dcp in range(8):
                    specs.append((w_ao_b, bw["w_ao_b"], D, dcp * 256, 256, o_consume(0, dcp)))
                    specs.append((w_in_b, bw["w_in_b"], D, C_GA + dcp * 256, 256, g_consume(0, dcp)))
                    specs.append((w_bo_b, bw["w_bo_b"], D, dcp * 256, 256, o_consume(1, dcp)))
                    specs.append((w_in_b, bw["w_in_b"], D, C_GB + dcp * 256, 256, g_consume(1, dcp)))

                def wout_consume(dcp):
                    def f(wt, bwt):
                        for cc in range(2):
                            dc = dcp * 2 + cc
                            pi = gp()
                            proj(pi, wt, bwt, KC, cc * 128, 128, lambda kc: mT[:, kc, :], [b_mT] + bq_heads["qAr"])
                            S.op(DVE, lambda: nc.vector.scalar_tensor_tensor(
                                out=xT[:, dc, :], in0=ps[pi][:], scalar=mod[:, G1 + dc, job:job + 1], in1=xT[:, dc, :],
                                op0=ALU.mult, op1=ALU.add), reads=[bps[pi], b_mod, b_xT], writes=[b_xT])
                    return f
                for dcp in range(8):
                    specs.append((w_out_b, bw["w_out_b"], D, dcp * 256, 256, wout_consume(dcp)))
                run_blocks(specs)

                layer_norm_inplace(xT, b_xT, 0, 1)
                S.dma(x1n_s[:, tok0:tok0 + TQ].rearrange("(kc p) n -> p kc n", p=128), xT[:], reads=[b_xT], writes=[b_x1n_s])

                for dc in range(KC):
                    h2f, bh2f = t32()
                    S.op(DVE, lambda dc=dc, h2f=h2f: nc.vector.tensor_scalar(
                        out=h2f[:], in0=xT[:, dc, :], scalar1=mod[:, SC2 + dc, job:job + 1], scalar2=mod[:, SH2 + dc, job:job + 1],
                        op0=ALU.mult, op1=ALU.add), reads=[b_xT, b_mod], writes=[bh2f])
                    S.op(PE, lambda dc=dc, h2f=h2f: nc.tensor.matmul(ps[6][0:72, :], w_r[:, dc, :], h2f[:], start=(dc == 0), stop=(dc == KC - 1)),
                         reads=[b_w_r, bh2f], writes=[bps[6]], signal=True)
                    S.op(ACT, lambda dc=dc, h2f=h2f: nc.scalar.copy(out=hT[:, dc, :], in_=h2f[:]), reads=[bh2f], writes=[b_hT])
                S.dma(h2_s[:, tok0:tok0 + TQ].rearrange("(kc p) n -> p kc n", p=128), hT[:], reads=[b_hT], writes=[b_h2_s])
                lt, blt = t32()
                S.op(ACT, lambda: nc.scalar.copy(out=lt[0:72, :], in_=ps[6][0:72, :]), reads=[bps[6]], writes=[blt])
                pc = 7
                for sub in range(4):
                    pi = gp()
                    S.op(PE, lambda: nc.tensor.transpose(out=ps[pi][:, 0:72], in_=lt[0:72, sub * 128:(sub + 1) * 128], identity=ident[0:72, 0:72]),
                         reads=[blt, b_ident], writes=[bps[pi]])
                    S.op(DVE, lambda: nc.vector.tensor_tensor(out=lg[:], in0=ps[pi][:, 0:72], in1=b_r[:], op=ALU.add),
                         reads=[bps[pi], b_b_r], writes=[b_lg])
                    (gmax, b_gmax), (ngmax, b_ngmax), (gsum, b_gsum), (pgrp, b_pgrp), (m1, b_m1), (m2_, b_m2), \
                        (dd, b_dd), (w1, b_w1), (w2, b_w2), (den, b_den) = sc
                    (ohg, b_ohg), (eg_, b_eg), (ein, b_ein), (oh1, b_oh1), (e2, b_e2), (oh2, b_oh2), (cwe, b_cwe), (junk, b_junk) = sm
                    S.op(DVE, lambda: nc.vector.tensor_reduce(out=gmax[:], in_=lg[:, 0:8], axis=AX.X, op=ALU.max), reads=[b_lg], writes=[b_gmax])
                    S.op(DVE, lambda: nc.vector.tensor_scalar(out=ohg[:], in0=lg[:, 0:8], scalar1=gmax[:, 0:1], scalar2=None, op0=ALU.is_equal),
                         reads=[b_lg, b_gmax], writes=[b_ohg])
                    S.op(DVE, lambda: nc.vector.tensor_scalar(out=ngmax[:], in0=gmax[:], scalar1=-1.0, scalar2=None, op0=ALU.mult),
                         reads=[b_gmax], writes=[b_ngmax])
                    S.op(DVE, lambda: nc.vector.memset(gsum[:], 0.0), writes=[b_gsum])
                    S.op(ACT, lambda: nc.scalar.activation(out=eg_[:], in_=lg[:, 0:8], func=AF.Exp, bias=ngmax[:], scale=1.0, accum_out=gsum[:]),
                         reads=[b_lg, b_ngmax], writes=[b_eg, b_gsum])
                    S.op(DVE, lambda: nc.vector.reciprocal(out=pgrp[:], in_=gsum[:]), reads=[b_gsum], writes=[b_pgrp])
                    for g in range(8):
                        if g == 0:
                            S.op(DVE, lambda: nc.vector.tensor_scalar(out=ein[:], in0=lg[:, 8:16], scalar1=ohg[:, 0:1], scalar2=None, op0=ALU.mult),
                                 reads=[b_lg, b_ohg], writes=[b_ein])
                        else:
                            S.op(DVE, lambda g=g: nc.vector.scalar_tensor_tensor(
                                out=ein[:], in0=lg[:, 8 + g * 8:16 + g * 8], scalar=ohg[:, g:g + 1], in1=ein[:], op0=ALU.mult, op1=ALU.add),
                                reads=[b_lg, b_ohg, b_ein], writes=[b_ein])
                    S.op(DVE, lambda: nc.vector.tensor_reduce(out=m1[:], in_=ein[:], axis=AX.X, op=ALU.max), reads=[b_ein], writes=[b_m1])
                    S.op(DVE, lambda: nc.vector.tensor_scalar(out=oh1[:], in0=ein[:], scalar1=m1[:, 0:1], scalar2=None, op0=ALU.is_equal),
                         reads=[b_ein, b_m1], writes=[b_oh1])
                    S.op(DVE, lambda: nc.vector.scalar_tensor_tensor(out=e2[:], in0=oh1[:], scalar=-1e30, in1=ein[:], op0=ALU.mult, op1=ALU.add),
                         reads=[b_oh1, b_ein], writes=[b_e2])
                    S.op(DVE, lambda: nc.vector.tensor_reduce(out=m2_[:], in_=e2[:], axis=AX.X, op=ALU.max), reads=[b_e2], writes=[b_m2])
                    S.op(DVE, lambda: nc.vector.tensor_scalar(out=oh2[:], in0=e2[:], scalar1=m2_[:, 0:1], scalar2=None, op0=ALU.is_equal),
                         reads=[b_e2, b_m2], writes=[b_oh2])
                    S.op(DVE, lambda: nc.vector.tensor_tensor(out=dd[:], in0=m2_[:], in1=m1[:], op=ALU.subtract), reads=[b_m1, b_m2], writes=[b_dd])
                    S.op(ACT, lambda: nc.scalar.activation(out=dd[:], in_=dd[:], func=AF.Exp), reads=[b_dd], writes=[b_dd])
                    S.op(DVE, lambda: nc.vector.tensor_scalar(out=den[:], in0=dd[:], scalar1=1.0, scalar2=None, op0=ALU.add), reads=[b_dd], writes=[b_den])
                    S.op(DVE, lambda: nc.vector.reciprocal(out=w1[:], in_=den[:]), reads=[b_den], writes=[b_w1])
                    S.op(DVE, lambda: nc.vector.tensor_tensor(out=w2[:], in0=dd[:], in1=w1[:], op=ALU.mult), reads=[b_dd, b_w1], writes=[b_w2])
                    S.op(DVE, lambda: nc.vector.tensor_tensor(out=w1[:], in0=w1[:], in1=pgrp[:], op=ALU.mult), reads=[b_w1, b_pgrp], writes=[b_w1])
                    S.op(DVE, lambda: nc.vector.tensor_tensor(out=w2[:], in0=w2[:], in1=pgrp[:], op=ALU.mult), reads=[b_w2, b_pgrp], writes=[b_w2])
                    S.op(DVE, lambda: nc.vector.tensor_scalar(out=cwe[:], in0=oh1[:], scalar1=w1[:, 0:1], scalar2=None, op0=ALU.mult),
                         reads=[b_oh1, b_w1], writes=[b_cwe])
                    S.op(DVE, lambda: nc.vector.scalar_tensor_tensor(out=cwe[:], in0=oh2[:], scalar=w2[:, 0:1], in1=cwe[:], op0=ALU.mult, op1=ALU.add),
                         reads=[b_oh2, b_w2, b_cwe], writes=[b_cwe])
                    for g in range(8):
                        S.op(DVE, lambda g=g: nc.vector.tensor_scalar(out=cw[:, g * 8:(g + 1) * 8], in0=cwe[:], scalar1=ohg[:, g:g + 1], scalar2=None, op0=ALU.mult),
                             reads=[b_cwe, b_ohg], writes=[b_cw])
                    S.op(PE, lambda: nc.tensor.transpose(out=ps[pc][0:64, sub * 128:(sub + 1) * 128], in_=cw[:], identity=ident[:]),
                         reads=[b_cw, b_ident], writes=[bps[pc]])
                S.op(ACT, lambda: nc.scalar.copy(out=cwT[:], in_=ps[pc][0:64, :]), reads=[bps[pc]], writes=[b_cwT])
                S.dma(cw_s[:, tok0:tok0 + TQ], cwT[:], reads=[b_cwT], writes=[b_cw_s])

            tok0 = 0
            tiles = []
            for job in range(2):
                phase_a(job)
            emit_casts(len(cast_list))
            for job in range(2):
                for t in range(NQ[job] // TQ):
                    phase_b(job, t, tok0)
                    tiles.append((job, t, tok0))
                    tok0 += TQ
        S.barrier()

        with ExitStack() as es2:
            h2m, b_h2m = sb(es2, "h2m", [128, KC, TQ], BF16)
            acc, b_acc = sb(es2, "acc", [128, KC, TQ], F32)
            wg = [sb(es2, f"wg{i}", [128, KC, DE], BF16) for i in range(2)]
            wu = [sb(es2, f"wu{i}", [128, KC, DE], BF16) for i in range(2)]
            wd = [sb(es2, f"wd{i}", [128, 4, D], BF16) for i in range(2)]
            hid, b_hid = sb(es2, "hid", [128, 4, TQ], BF16)
            cwb = [sb(es2, f"cwb{i}", [128, TQ], F32) for i in range(3)]
            sgs = [sb(es2, f"sgs{i}", [128, TQ], F32) for i in range(3)]
            xa = [sb(es2, f"xa{i}", [128, TQ], F32) for i in range(2)]
            rstd2, b_rstd2 = sb(es2, "rstd2", [128, TQ], F32)
            mean2, b_mean2 = sb(es2, "mean2", [128, TQ], F32)
            m2t, b_m2t = sb(es2, "m2t", [128, TQ], F32)
            l16 = [sb(es2, f"l16_{i}", [128, TQ], BF16) for i in range(4)]

            def load_expert(e, tok0, k):
                i = k % 2
                S.dma(wg[i][0][:], eg_b[e].rearrange("(kc p) n -> p kc n", p=128), reads=[bw["e_b"]], writes=[wg[i][1]])
                S.dma(wu[i][0][:], eu_b[e].rearrange("(kc p) n -> p kc n", p=128), reads=[bw["e_b"]], writes=[wu[i][1]])
                S.dma(wd[i][0][:], ed_b[e].rearrange("(kc p) n -> p kc n", p=128), reads=[bw["e_b"]], writes=[wd[i][1]])
                S.dma(cwb[k % 3][0][:], cw_s[e:e + 1, tok0:tok0 + TQ].partition_broadcast(128), reads=[b_cw_s], writes=[cwb[k % 3][1]])

            k = 0
            for (job, t, tok0) in tiles:
                S.dma(h2m[:], h2_s[:, tok0:tok0 + TQ].rearrange("(kc p) n -> p kc n", p=128), reads=[b_h2_s], writes=[b_h2m])
                load_expert(0, tok0, k)
                for e in range(NE):
                    if e + 1 < NE:
                        load_expert(e + 1, tok0, k + 1)
                    i = k % 2
                    wgt, bwg = wg[i]
                    wut, bwu = wu[i]
                    wdt, bwd = wd[i]
                    cwt_, bcw = cwb[k % 3]
                    for hc in range(4):
                        pa = gp()
                        for kc in range(KC):
                            S.op(PE, lambda kc=kc: nc.tensor.matmul(ps[pa][:], wgt[:, kc, hc * 128:(hc + 1) * 128], h2m[:, kc, :],
                                                                   start=(kc == 0), stop=(kc == KC - 1)),
                                 reads=[bwg, b_h2m], writes=[bps[pa]], signal=(kc == KC - 1))
                        pb = gp()
                        for kc in range(KC):
                            S.op(PE, lambda kc=kc: nc.tensor.matmul(ps[pb][:], wut[:, kc, hc * 128:(hc + 1) * 128], h2m[:, kc, :],
                                                                   start=(kc == 0), stop=(kc == KC - 1)),
                                 reads=[bwu, b_h2m], writes=[bps[pb]], signal=(kc == KC - 1))
                        sg, bsg = sgs[(k * 4 + hc) % 3]
                        S.op(ACT, lambda: nc.scalar.activation(out=sg[:], in_=ps[pa][:], func=AF.Silu), reads=[bps[pa]], writes=[bsg])
                        S.op(POOL, lambda: nc.gpsimd.tensor_tensor(out=sg[:], in0=sg[:], in1=cwt_[:], op=ALU.mult), reads=[bsg, bcw], writes=[bsg])
                        S.op(DVE, lambda: nc.vector.tensor_tensor(out=hid[:, hc, :], in0=ps[pb][:], in1=sg[:], op=ALU.mult),
                             reads=[bps[pb], bsg], writes=[b_hid])
                    for dc in range(KC):
                        pd = 4 + (dc % 4)
                        for kc in range(4):
                            S.op(PE, lambda kc=kc: nc.tensor.matmul(ps[pd][:], wdt[:, kc, dc * 128:(dc + 1) * 128], hid[:, kc, :],
                                                                   start=(kc == 0), stop=(kc == 3)),
                                 reads=[bwd, b_hid], writes=[bps[pd]], signal=(kc == 3))
                        if e == 0:
                            S.op(DVE, lambda: nc.vector.tensor_copy(acc[:, dc, :], ps[pd][:]), reads=[bps[pd]], writes=[b_acc])
                        else:
                            S.op(DVE, lambda: nc.vector.tensor_tensor(out=acc[:, dc, :], in0=ps[pd][:], in1=acc[:, dc, :], op=ALU.add),
                                 reads=[bps[pd], b_acc], writes=[b_acc])
                    k += 1
                for dc in range(KC):
                    xt_, bxt = xa[dc % 2]
                    S.dma(xt_[:], x1n_s[dc * 128:(dc + 1) * 128, tok0:tok0 + TQ], reads=[b_x1n_s], writes=[bxt])
                    S.op(DVE, lambda: nc.vector.tensor_scalar(out=acc[:, dc, :], in0=acc[:, dc, :], scalar1=mod[:, G2 + dc, job:job + 1],
                                                              scalar2=None, op0=ALU.mult), reads=[b_acc, b_mod], writes=[b_acc])
                    S.op(DVE, lambda: nc.vector.scalar_tensor_tensor(out=acc[:, dc, :], in0=xt_[:], scalar=ALPHA, in1=acc[:, dc, :],
                                                                     op0=ALU.mult, op1=ALU.add), reads=[bxt, b_acc], writes=[b_acc])
                for dc in range(KC):
                    xb, bxb = l16[(2 * dc) % 4]
                    sq, bsq = l16[(2 * dc + 1) % 4]
                    S.op(ACT, lambda: nc.scalar.copy(out=xb[:], in_=acc[:, dc, :]), reads=[b_acc], writes=[bxb])
                    S.op(ACT, lambda: nc.scalar.activation(out=sq[:], in_=acc[:, dc, :], func=AF.Square), reads=[b_acc], writes=[bsq])
                    S.op(PE, lambda: nc.tensor.matmul(ps[0][:], onesD_bf[:], xb[:], start=(dc == 0), stop=(dc == KC - 1)),
                         reads=[b_onesD, bxb], writes=[bps[0]], signal=True)
                    S.op(PE, lambda: nc.tensor.matmul(ps[1][:], onesD_bf[:], sq[:], start=(dc == 0), stop=(dc == KC - 1)),
                         reads=[b_onesD, bsq], writes=[bps[1]], signal=True)
                S.op(ACT, lambda: nc.scalar.copy(out=mean2[:], in_=ps[0][:]), reads=[bps[0]], writes=[b_mean2])
                S.op(DVE, lambda: nc.vector.tensor_tensor(out=m2t[:], in0=mean2[:], in1=mean2[:], op=ALU.mult), reads=[b_mean2], writes=[b_m2t])
                S.op(DVE, lambda: nc.vector.tensor_tensor(out=rstd2[:], in0=ps[1][:], in1=m2t[:], op=ALU.subtract), reads=[bps[1], b_m2t], writes=[b_rstd2])
                S.op(ACT, lambda: nc.scalar.activation(out=rstd2[:], in_=rstd2[:], func=AF.Sqrt, bias=epsl[:], scale=1.0),
                     reads=[b_rstd2, b_epsl], writes=[b_rstd2])
                S.op(DVE, lambda: nc.vector.reciprocal(out=rstd2[:], in_=rstd2[:]), reads=[b_rstd2], writes=[b_rstd2])
                for dc in range(KC):
                    S.op(POOL, lambda: nc.gpsimd.tensor_tensor(out=acc[:, dc, :], in0=acc[:, dc, :], in1=mean2[:], op=ALU.subtract),
                         reads=[b_acc, b_mean2], writes=[b_acc])
                    S.op(DVE, lambda: nc.vector.tensor_tensor(out=acc[:, dc, :], in0=acc[:, dc, :], in1=rstd2[:], op=ALU.mult),
                         reads=[b_acc, b_rstd2], writes=[b_acc])
                    S.op(DVE, lambda: nc.vector.tensor_scalar(out=acc[:, dc, :], in0=acc[:, dc, :], scalar1=lngb[:, 2, dc:dc + 1],
                                                              scalar2=lngb[:, 3, dc:dc + 1], op0=ALU.mult, op1=ALU.add),
                         reads=[b_acc, b_lngb], writes=[b_acc])
                b_y = Buf("y")
                S.dma(y_d[job][:, t * TQ:(t + 1) * TQ].rearrange("(kc p) n -> p kc n", p=128), acc[:], reads=[b_acc], writes=[b_y])
                S._waits(S.SP, [b_y], [])
        S.barrier()
    return nc


def _rope_tables(S):
    inv = (THETA ** (-np.arange(0, 64, 2, dtype=np.float32) / np.float32(64))).astype(np.float32)
    t = np.arange(S, dtype=np.int32)
    row = (t // 64).astype(np.float32)
    col = (t % 64).astype(np.float32)
    ang_a = t.astype(np.float32)[:, None] * inv[None, :]
    ang_r = row[:, None] * inv[None, :]
    ang_c = col[:, None] * inv[None, :]
    tA = np.empty((2, 64, S), np.float32)
    tB = np.empty((2, 128, S), np.float32)
    for k, fn in enumerate((np.cos, np.sin)):
        a = fn(ang_a).astype(np.float32).T
        tA[k, 0:32] = a
        tA[k, 32:64] = a
        r = fn(ang_r).astype(np.float32).T
        c = fn(ang_c).astype(np.float32).T
        tB[k, 0:32] = r
        tB[k, 32:64] = r
        tB[k, 64:96] = c
        tB[k, 96:128] = c
    return tA, tB


def _fm(v, kc):
    return np.ascontiguousarray(np.asarray(v, np.float32).reshape(kc, 128).T)


def run(inputs, n_prompt, s_prompt, s_sample):
    f = lambda k: np.asarray(inputs[k], np.float32)
    xp, xs = f("x_prompt"), f("x_sample")
    NKP, NKS = s_prompt, s_sample
    NQP, NQS = s_prompt // 2, s_sample // 8
    nc = build(NKP, NQP, NKS, NQS)

    tAp, tBp = _rope_tables(NKP)
    tAs, tBs = _rope_tables(NKS)
    RA = np.zeros((64, 64), np.float32)
    for i in range(32):
        RA[i + 32, i] = -1.0
        RA[i, i + 32] = 1.0
    RB = np.zeros((128, 128), np.float32)
    RB[0:64, 0:64] = RA
    RB[64:128, 64:128] = RA
    shared = {
        "RA": RA, "RB": RB, "ident": np.eye(128, dtype=np.float32),
        "w_ada": f("w_ada")[0], "b_adaT": _fm(f("b_ada")[0], 96),
        "w_in": f("w_in")[0], "a_q_norm": _fm(f("a_q_norm")[0], 4), "a_kv_norm": _fm(f("a_kv_norm")[0], 4),
        "a_w_uq": f("a_w_uq")[0], "a_w_ukv": f("a_w_ukv")[0], "a_w_o": f("a_w_o")[0],
        "b_q_norm": f("b_q_norm")[0].reshape(128, 1), "b_k_norm": f("b_k_norm")[0].reshape(128, 1),
        "b_w_o": f("b_w_o")[0], "w_out": f("w_out")[0],
        "ln_gb": np.ascontiguousarray(np.stack([_fm(f("ln1_g")[0], 16), _fm(f("ln1_b")[0], 16),
                                                _fm(f("ln2_g")[0], 16), _fm(f("ln2_b")[0], 16)], axis=1)),
        "w_r": np.ascontiguousarray(np.concatenate([f("w_group")[0], f("w_expert")[0]], axis=1)),
        "b_r": np.ascontiguousarray(np.broadcast_to(np.concatenate([f("b_group")[0], f("b_expert")[0]])[None, :], (128, 72))),
        "e_w_gate": f("e_w_gate")[0], "e_w_up": f("e_w_up")[0], "e_w_down": f("e_w_down")[0],
    }
    xpT = [np.ascontiguousarray(xp[b].T) for b in range(n_prompt)]
    xsT = np.ascontiguousarray(xs[0].T)
    cp, cs_ = f("c_prompt"), f("c_sample")
    in_maps = []
    for c in range(8):
        b, half = c // 2, c % 2
        b = min(b, n_prompt - 1)
        cT = np.stack([cp[b], cs_[0]], axis=1)
        cT = np.ascontiguousarray(cT.reshape(KC, 128, 2).transpose(1, 0, 2))
        m = dict(shared)
        m.update({
            "xkp": xpT[b], "xks": xsT,
            "xqp": np.ascontiguousarray(xpT[b][:, half * NQP:(half + 1) * NQP]),
            "xqs": np.ascontiguousarray(xsT[:, c * NQS:(c + 1) * NQS]),
            "tAkp": tAp, "tBkp": tBp, "tAks": tAs, "tBks": tBs,
            "tAqp": np.ascontiguousarray(tAp[:, :, half * NQP:(half + 1) * NQP]),
            "tBqp": np.ascontiguousarray(tBp[:, :, half * NQP:(half + 1) * NQP]),
            "tAqs": np.ascontiguousarray(tAs[:, :, c * NQS:(c + 1) * NQS]),
            "tBqs": np.ascontiguousarray(tBs[:, :, c * NQS:(c + 1) * NQS]),
            "cT": cT,
        })
        in_maps.append(m)
    res = run_bass_kernel_spmd(nc, in_maps, core_ids=list(range(8)))
    y_p = np.empty((n_prompt, s_prompt, D), np.float32)
    y_s = np.empty((1, s_sample, D), np.float32)
    for c in range(8):
        b, half = c // 2, c % 2
        r = res.results[c]
        if b < n_prompt:
            y_p[b, half * NQP:(half + 1) * NQP, :] = np.asarray(r["yp"]).T
        y_s[0, c * NQS:(c + 1) * NQS, :] = np.asarray(r["ys"]).T
    return y_p, y_s


def kernel(**inputs):
    return run(inputs, 4, 8192, 16384)
```

```python
import numpy as np
from contextlib import ExitStack
import concourse.bass as bass
import concourse.mybir as mybir
from concourse.bass_utils import run_bass_kernel_spmd

F32 = mybir.dt.float32
BF16 = mybir.dt.bfloat16
AF = mybir.ActivationFunctionType
ALU = mybir.AluOpType
AX = mybir.AxisListType

D = 2048
KC = 16
NH = 16
NKVH = 4
NE = 64
DE = 512
TQ = 512
ALPHA = 2.0 ** 0.25
RMS_EPS = 1e-6
LN_EPS = 1e-5
THETA = 10000.0
C_CQ, C_CKV, C_KR, C_QB, C_KB, C_VB, C_GA, C_GB = 0, 512, 1024, 1088, 3136, 3648, 4160, 6208
D_IN = 8256


class Tok:
    def __init__(self, sem, name):
        self.sem = sem
        self.name = name
        self.count = 0


class Eng(Tok):
    def __init__(self, eng, sem, name, strict_self):
        super().__init__(sem, name)
        self.eng = eng
        self.waited = {}
        self.strict_self = strict_self


class Buf:
    __slots__ = ("name", "w", "r", "multi")

    def __init__(self, name, multi=False):
        self.name = name
        self.w = {}
        self.r = {}
        self.multi = multi


class Sched:
    def __init__(self, nc, es, n_dma_sems=24):
        self.nc = nc

        def mk(eng, name, strict):
            return Eng(eng, es.enter_context(nc.semaphore("s_" + name)), name, strict)

        self.PE = mk(nc.tensor, "pe", False)
        self.ACT = mk(nc.scalar, "act", True)
        self.DVE = mk(nc.vector, "dve", True)
        self.POOL = mk(nc.gpsimd, "pool", True)
        self.SP = mk(nc.sync, "sp", False)
        self.engs = [self.PE, self.ACT, self.DVE, self.POOL, self.SP]
        self.dma_toks = {}
        for q in ("sp", "pool"):
            self.dma_toks[q] = [Tok(es.enter_context(nc.semaphore(f"d_{q}{i}")), f"d_{q}{i}")
                                for i in range(n_dma_sems)]
        self.dma_rr = {"sp": 0, "pool": 0}
        self.n_inst = 0

    def _waits(self, E, reads, writes):
        need = {}
        for b in reads:
            for t, v in b.w.items():
                if need.get(t, 0) < v:
                    need[t] = v
        for b in writes:
            if b.multi:
                continue
            for t, v in b.w.items():
                if need.get(t, 0) < v:
                    need[t] = v
            for t, v in b.r.items():
                if need.get(t, 0) < v:
                    need[t] = v
        for t, v in need.items():
            if t is E and not E.strict_self:
                continue
            if E.waited.get(t, 0) >= v:
                continue
            E.eng.wait_ge(t.sem, v)
            E.waited[t] = v
            self.n_inst += 1

    def _mark(self, tok, mark, reads, writes):
        for b in reads:
            if b.r.get(tok, 0) < mark:
                b.r[tok] = mark
        for b in writes:
            if b.multi:
                if b.w.get(tok, 0) < mark:
                    b.w[tok] = mark
            else:
                b.w = {tok: mark}
                b.r = {}

    def op(self, E, fn, reads=(), writes=(), signal=True):
        self._waits(E, reads, writes)
        ins = fn()
        self.n_inst += 1
        if signal:
            E.count += 1
            ins.then_inc(E.sem, 1)
            mark = E.count
        else:
            mark = E.count + 1
        self._mark(E, mark, reads, writes)
        return ins

    def dma(self, out_ap, in_ap, reads=(), writes=(), q="sp"):
        E = self.SP if q == "sp" else self.POOL
        toks = self.dma_toks[q]
        t = toks[self.dma_rr[q] % len(toks)]
        self.dma_rr[q] += 1
        if t.count > 0 and E.waited.get(t, 0) < t.count:
            E.eng.wait_ge(t.sem, t.count)
            E.waited[t] = t.count
        self._waits(E, reads, writes)
        ins = E.eng.dma_start(out=out_ap, in_=in_ap)
        t.count += 16
        ins.then_inc(t.sem, 16)
        self.n_inst += 1
        self._mark(t, t.count, reads, writes)
        return ins

    def barrier(self):
        toks = list(self.engs)
        for q in self.dma_toks.values():
            toks += q
        for E in self.engs:
            for t in toks:
                if t is E or t.count == 0 or E.waited.get(t, 0) >= t.count:
                    continue
                E.eng.wait_ge(t.sem, t.count)
                E.waited[t] = t.count


def build(NKP, NQP, NKS, NQS):
    T = NQP + NQS
    nc = bass.Bass("TRN2", target_bir_lowering=False)

    def din(name, shape, dt=F32):
        return nc.dram_tensor(name, list(shape), dt, kind="ExternalInput").ap()

    def dscr(name, shape, dt):
        return nc.dram_tensor(name, list(shape), dt).ap()

    NK = [NKP, NKS]
    NQ = [NQP, NQS]
    xk = [din("xkp", [D, NKP]), din("xks", [D, NKS])]
    xq = [din("xqp", [D, NQP]), din("xqs", [D, NQS])]
    tAk = [din("tAkp", [2, 64, NKP]), din("tAks", [2, 64, NKS])]
    tBk = [din("tBkp", [2, 128, NKP]), din("tBks", [2, 128, NKS])]
    tAq = [din("tAqp", [2, 64, NQP]), din("tAqs", [2, 64, NQS])]
    tBq = [din("tBqp", [2, 128, NQP]), din("tBqs", [2, 128, NQS])]
    cT_d = din("cT", [128, KC, 2])
    RA_d = din("RA", [64, 64])
    RB_d = din("RB", [128, 128])
    ident_d = din("ident", [128, 128])
    w_ada_d = din("w_ada", [D, 6 * D])
    b_ada_d = din("b_adaT", [128, 96])
    w_in_d = din("w_in", [D, D_IN])
    aqn_d = din("a_q_norm", [128, 4])
    akvn_d = din("a_kv_norm", [128, 4])
    w_uq_d = din("a_w_uq", [512, 3072])
    w_ukv_d = din("a_w_ukv", [512, 4096])
    w_ao_d = din("a_w_o", [D, D])
    bqn_d = din("b_q_norm", [128, 1])
    bkn_d = din("b_k_norm", [128, 1])
    w_bo_d = din("b_w_o", [D, D])
    w_out_d = din("w_out", [D, D])
    ln_d = din("ln_gb", [128, 4, KC])
    w_r_d = din("w_r", [D, 72])
    b_r_d = din("b_r", [128, 72])
    eg_d = din("e_w_gate", [NE, D, DE])
    eu_d = din("e_w_up", [NE, D, DE])
    ed_d = din("e_w_down", [NE, DE, D])
    y_d = [nc.dram_tensor("yp", [D, NQP], F32, kind="ExternalOutput").ap(),
           nc.dram_tensor("ys", [D, NQS], F32, kind="ExternalOutput").ap()]

    w_in_b = dscr("w_in_b", [D, D_IN], BF16)
    w_uq_b = dscr("w_uq_b", [512, 3072], BF16)
    w_ukv_b = dscr("w_ukv_b", [512, 4096], BF16)
    w_ao_b = dscr("w_ao_b", [D, D], BF16)
    w_bo_b = dscr("w_bo_b", [D, D], BF16)
    w_out_b = dscr("w_out_b", [D, D], BF16)
    eg_b = dscr("eg_b", [NE, D, DE], BF16)
    eu_b = dscr("eu_b", [NE, D, DE], BF16)
    ed_b = dscr("ed_b", [NE, DE, D], BF16)
    KA = [dscr(f"KA{j}", [NH, 128, NK[j]], BF16) for j in range(2)]
    KR = [dscr(f"KR{j}", [64, NK[j]], BF16) for j in range(2)]
    VA = [dscr(f"VA{j}", [NH, NK[j], 128], BF16) for j in range(2)]
    KB = [dscr(f"KB{j}", [NKVH, 128, NK[j]], BF16) for j in range(2)]
    VB = [dscr(f"VB{j}", [NKVH, NK[j], 128], BF16) for j in range(2)]
    x1n_s = dscr("x1n_s", [D, T], F32)
    h2_s = dscr("h2_s", [D, T], BF16)
    cw_s = dscr("cw_s", [NE, T], F32)

    with ExitStack() as es:
        S = Sched(nc, es)
        PE, ACT, DVE, POOL = S.PE, S.ACT, S.DVE, S.POOL

        def sb(stack, name, shape, dt):
            return stack.enter_context(nc.sbuf_tensor("sb_" + name, list(shape), dt)), Buf(name)

        ps = [es.enter_context(nc.psum_tensor(f"ps{i}", [128, 512], F32)) for i in range(8)]
        bps = [Buf(f"ps{i}") for i in range(8)]
        gp_state = [0]

        def gp():
            i = gp_state[0] % 4
            gp_state[0] += 1
            return i

        ones_bf, b_ones = sb(es, "ones_bf", [128, 128], BF16)
        onesD_bf, b_onesD = sb(es, "onesD_bf", [128, 128], BF16)
        RA_bf, b_RA = sb(es, "RA_bf", [64, 64], BF16)
        RB_bf, b_RB = sb(es, "RB_bf", [128, 128], BF16)
        ident, b_ident = sb(es, "ident", [128, 128], F32)
        epsr, b_epsr = sb(es, "epsr", [128, 1], F32)
        epsl, b_epsl = sb(es, "epsl", [128, 1], F32)
        mod, b_mod = sb(es, "mod", [128, 96, 2], F32)
        aqn, b_aqn = sb(es, "aqn", [128, 4], F32)
        akvn, b_akvn = sb(es, "akvn", [128, 4], F32)
        bqn, b_bqn = sb(es, "bqn", [128, 1], F32)
        bkn, b_bkn = sb(es, "bkn", [128, 1], F32)
        lngb, b_lngb = sb(es, "lngb", [128, 4, KC], F32)
        w_r, b_w_r = sb(es, "w_r", [128, KC, 72], F32)
        b_r, b_b_r = sb(es, "b_r", [128, 72], F32)
        stage, b_stage = sb(es, "stage", [128, 128], F32)
        ones_f, b_ones_f = sb(es, "ones_f", [128, 128], F32)

        S.op(DVE, lambda: nc.vector.memset(ones_bf[:], 1.0), writes=[b_ones])
        S.op(DVE, lambda: nc.vector.memset(ones_f[:], 1.0), writes=[b_ones_f])
        S.op(DVE, lambda: nc.vector.memset(onesD_bf[:], 1.0 / D), writes=[b_onesD])
        S.op(DVE, lambda: nc.vector.memset(epsr[:], RMS_EPS), writes=[b_epsr])
        S.op(DVE, lambda: nc.vector.memset(epsl[:], LN_EPS), writes=[b_epsl])
        S.dma(stage[:], RB_d, writes=[b_stage])
        S.op(DVE, lambda: nc.vector.tensor_copy(RB_bf[:], stage[:]), reads=[b_stage], writes=[b_RB])
        S.dma(stage[0:64, 0:64], RA_d, writes=[b_stage])
        S.op(DVE, lambda: nc.vector.tensor_copy(RA_bf[:], stage[0:64, 0:64]), reads=[b_stage], writes=[b_RA])
        S.dma(ident[:], ident_d, writes=[b_ident])
        S.dma(aqn[:], aqn_d, writes=[b_aqn])
        S.dma(akvn[:], akvn_d, writes=[b_akvn])
        S.dma(bqn[:], bqn_d, writes=[b_bqn])
        S.dma(bkn[:], bkn_d, writes=[b_bkn])
        S.dma(lngb[:], ln_d, writes=[b_lngb])
        S.dma(w_r[:], w_r_d.rearrange("(kc p) n -> p kc n", p=128), writes=[b_w_r])
        S.dma(b_r[:], b_r_d, writes=[b_b_r])

        bw = {k: Buf(k, multi=True) for k in ("w_in_b", "w_uq_b", "w_ukv_b", "w_ao_b", "w_bo_b", "w_out_b", "e_b")}
        R4 = D // 4
        for i in range(4):
            S.dma(w_in_b[i * R4:(i + 1) * R4, :], w_in_d[i * R4:(i + 1) * R4, :], writes=[bw["w_in_b"]], q="pool")
        S.dma(w_uq_b, w_uq_d, writes=[bw["w_uq_b"]], q="pool")
        S.dma(w_ukv_b, w_ukv_d, writes=[bw["w_ukv_b"]], q="pool")
        S.dma(w_ao_b, w_ao_d, writes=[bw["w_ao_b"]], q="pool")
        S.dma(w_bo_b, w_bo_d, writes=[bw["w_bo_b"]], q="pool")
        S.dma(w_out_b, w_out_d, writes=[bw["w_out_b"]], q="pool")

        with ExitStack() as es0:
            cs, b_cs = sb(es0, "cs", [128, KC, 2], F32)
            badaT, b_badaT = sb(es0, "badaT", [128, 96], F32)
            wa = [sb(es0, f"wa{i}", [128, KC, 128], F32) for i in range(3)]
            S.dma(cs[:], cT_d, writes=[b_cs])
            S.dma(badaT[:], b_ada_d, writes=[b_badaT])
            S.op(ACT, lambda: nc.scalar.activation(out=cs[:], in_=cs[:], func=AF.Silu), reads=[b_cs], writes=[b_cs])
            wav = w_ada_d.rearrange("(kc p) n -> p kc n", p=128)
            for j in range(96 + 2):
                if j < 96:
                    t_, b_ = wa[j % 3]
                    S.dma(t_[:], wav[:, :, j * 128:(j + 1) * 128], writes=[b_])
                jj = j - 2
                if jj >= 0:
                    t_, b_ = wa[jj % 3]
                    pi = gp()
                    for kc in range(KC):
                        S.op(PE, lambda t_=t_, kc=kc, pi=pi: nc.tensor.matmul(
                            ps[pi][:, 0:2], t_[:, kc, :], cs[:, kc, :], start=(kc == 0), stop=(kc == KC - 1)),
                            reads=[b_, b_cs], writes=[bps[pi]], signal=(kc == KC - 1))
                    S.op(DVE, lambda jj=jj, pi=pi: nc.vector.tensor_scalar(
                        out=mod[:, jj, :], in0=ps[pi][:, 0:2], scalar1=badaT[:, jj:jj + 1], scalar2=None, op0=ALU.add),
                        reads=[bps[pi], b_badaT], writes=[b_mod])
            for lo in (16, 64):
                S.op(DVE, lambda lo=lo: nc.vector.tensor_scalar(
                    out=mod[:, lo:lo + 16, :], in0=mod[:, lo:lo + 16, :], scalar1=1.0, scalar2=None, op0=ALU.add),
                    reads=[b_mod], writes=[b_mod])
        S.barrier()

        cast_list = []
        for e in range(NE):
            cast_list += [(eg_b[e], eg_d[e]), (eu_b[e], eu_d[e]), (ed_b[e], ed_d[e])]
        n_a_tiles = (NKP + NKS) // TQ
        casts_per_tile = -(-len(cast_list) // n_a_tiles)

        def emit_casts(n):
            for _ in range(n):
                if cast_list:
                    d_, s_ = cast_list.pop(0)
                    S.dma(d_, s_, writes=[bw["e_b"]], q="pool")

        SH1, SC1, G1, SH2, SC2, G2 = 0, 16, 32, 48, 64, 80
        bKV = [Buf("kv0", multi=True), Buf("kv1", multi=True)]
        b_x1n_s, b_h2_s, b_cw_s = Buf("x1n_s", multi=True), Buf("h2_s", multi=True), Buf("cw_s", multi=True)

        with ExitStack() as es1:
            xT, b_xT = sb(es1, "xT", [128, KC, TQ], F32)
            hT, b_hT = sb(es1, "hT", [128, KC, TQ], BF16)
            qAn, b_qAn = sb(es1, "qAn", [128, NH, TQ], BF16)
            qAr, b_qAr = sb(es1, "qAr", [128, NH, TQ], BF16)
            qB, b_qB = sb(es1, "qB", [128, NH, TQ], BF16)
            bq_heads = {"qAn": [Buf(f"qAn{h}") for h in range(NH)], "qAr": [Buf(f"qAr{h}") for h in range(NH)],
                        "qB": [Buf(f"qB{h}") for h in range(NH)]}
            NWS = 3
            wslot = [sb(es1, f"wslot{i}", [128, KC, 256], BF16) for i in range(NWS)]
            ws_state = [0]
            KCH = 1024
            NKS_ = 3
            kts = [sb(es1, f"kt{i}", [128, KCH], BF16) for i in range(NKS_)]
            krs = [sb(es1, f"kr{i}", [64, KCH], BF16) for i in range(NKS_)]
            vvs = [sb(es1, f"vv{i}", [128, KCH // 128, 128], BF16) for i in range(NKS_)]
            pTs = [sb(es1, f"pT{i}", [128, 512], BF16) for i in range(4)]
            paccs = [[sb(es1, f"pacc{a}_{i}", [128, 512], F32) for i in range(3)] for a in range(2)]
            craw, b_craw = sb(es1, "craw", [128, 4, TQ], F32)
            cn, b_cn = sb(es1, "cn", [128, 4, TQ], BF16)
            tcA, b_tcA = sb(es1, "tcA", [64, 2, TQ], F32)
            tcB, b_tcB = sb(es1, "tcB", [128, 2, TQ], F32)
            rstd, b_rstd = sb(es1, "rstd", [128, TQ], F32)
            mean, b_mean = sb(es1, "mean", [128, TQ], F32)
            tmp32 = [sb(es1, f"tmp32_{i}", [128, TQ], F32) for i in range(5)]
            tmp16 = [sb(es1, f"tmp16_{i}", [128, TQ], BF16) for i in range(4)]
            t32_state = [0]
            t16_state = [0]
            lg, b_lg = sb(es1, "lg", [128, 72], F32)
            sm = [sb(es1, f"sm{i}", [128, 8], F32) for i in range(8)]
            sc = [sb(es1, f"sc{i}", [128, 1], F32) for i in range(10)]
            cw, b_cw = sb(es1, "cw", [128, NE], F32)
            cwT, b_cwT = sb(es1, "cwT", [64, TQ], F32)

            def t32():
                i = t32_state[0] % len(tmp32)
                t32_state[0] += 1
                return tmp32[i]

            def t16():
                i = t16_state[0] % len(tmp16)
                t16_state[0] += 1
                return tmp16[i]

            def load_w(src, bsrc, krows, c0, ncols):
                i = ws_state[0] % NWS
                ws_state[0] += 1
                t_, b_ = wslot[i]
                kcn = krows // 128
                S.dma(t_[:, 0:kcn, 0:ncols], src.rearrange("(kc p) n -> p kc n", p=128)[:, :, c0:c0 + ncols],
                      reads=[bsrc], writes=[b_])
                return t_, b_

            def run_blocks(specs, pre=2):
                loaded = []
                for i in range(len(specs) + pre):
                    if i < len(specs):
                        sp_ = specs[i]
                        loaded.append(load_w(sp_[0], sp_[1], sp_[2], sp_[3], sp_[4]))
                    j = i - pre
                    if j >= 0:
                        specs[j][5](*loaded[j])

            def proj(pi, wt, bwt, kcn, c0, m, rhs_fn, rhs_bufs, prow=128):
                for kc in range(kcn):
                    S.op(PE, lambda kc=kc: nc.tensor.matmul(
                        ps[pi][0:m, :], wt[0:prow, kc, c0:c0 + m], rhs_fn(kc), start=(kc == 0), stop=(kc == kcn - 1)),
                        reads=[bwt] + rhs_bufs, writes=[bps[pi]], signal=(kc == kcn - 1))

            def load_x_tile(src, t0, job):
                S.dma(xT[:], src.rearrange("(kc p) n -> p kc n", p=128)[:, :, t0:t0 + TQ], writes=[b_xT])
                for kc in range(KC):
                    S.op(DVE, lambda kc=kc: nc.vector.tensor_scalar(
                        out=hT[:, kc, :], in0=xT[:, kc, :], scalar1=mod[:, SC1 + kc, job:job + 1],
                        scalar2=mod[:, SH1 + kc, job:job + 1], op0=ALU.mult, op1=ALU.add),
                        reads=[b_xT, b_mod], writes=[b_hT])

            def load_tables(tA, tB, t0):
                S.dma(tcA[:], tA.rearrange("c p n -> p c n")[:, :, t0:t0 + TQ], writes=[b_tcA])
                S.dma(tcB[:], tB.rearrange("c p n -> p c n")[:, :, t0:t0 + TQ], writes=[b_tcB])

            def rms_lowrank(specs_cols, gain, c_off):
                def consume(blk):
                    def f(wt, bwt):
                        for cc in range(2):
                            c = blk * 2 + cc
                            pi = gp()
                            proj(pi, wt, bwt, KC, cc * 128, 128, lambda kc: hT[:, kc, :], [b_hT])
                            S.op(ACT, lambda: nc.scalar.copy(out=craw[:, c, :], in_=ps[pi][:]),
                                 reads=[bps[pi]], writes=[b_craw])
                            sq, bsq = t16()
                            S.op(ACT, lambda: nc.scalar.activation(out=sq[:], in_=ps[pi][:], func=AF.Square),
                                 reads=[bps[pi]], writes=[bsq])
                            S.op(PE, lambda: nc.tensor.matmul(ps[6][:], ones_bf[:], sq[:], start=(c == 0), stop=(c == 3)),
                                 reads=[b_ones, bsq], writes=[bps[6]], signal=True)
                    return f
                run_blocks([(w_in_b, bw["w_in_b"], D, c_off + blk * 256, 256, consume(blk)) for blk in range(2)])
                S.op(ACT, lambda: nc.scalar.activation(out=rstd[:], in_=ps[6][:], func=AF.Sqrt, bias=epsr[:], scale=1.0 / 512),
                     reads=[bps[6], b_epsr], writes=[b_rstd])
                S.op(DVE, lambda: nc.vector.reciprocal(out=rstd[:], in_=rstd[:]), reads=[b_rstd], writes=[b_rstd])
                for c in range(4):
                    S.op(DVE, lambda c=c: nc.vector.scalar_tensor_tensor(
                        out=cn[:, c, :], in0=craw[:, c, :], scalar=gain[0][:, c:c + 1], in1=rstd[:], op0=ALU.mult, op1=ALU.mult),
                        reads=[b_craw, gain[1], b_rstd], writes=[b_cn])

            def rope_finish(pi_raw, raw16, braw16, R_bf, bR, tab, btab, rows, out_ap, out_bufs):
                pj = gp()
                S.op(PE, lambda: nc.tensor.matmul(ps[pj][0:rows, :], R_bf[:], raw16[0:rows, :], start=True, stop=True),
                     reads=[bR, braw16], writes=[bps[pj]])
                ta, bta = t32()
                tb, btb = t32()
                S.op(DVE, lambda: nc.vector.tensor_tensor(out=ta[0:rows, :], in0=raw16[0:rows, :], in1=tab[0:rows, 0, :], op=ALU.mult),
                     reads=[braw16, btab], writes=[bta])
                S.op(DVE, lambda: nc.vector.tensor_tensor(out=tb[0:rows, :], in0=ps[pj][0:rows, :], in1=tab[0:rows, 1, :], op=ALU.mult),
                     reads=[bps[pj], btab], writes=[btb])
                S.op(DVE, lambda: nc.vector.tensor_tensor(out=out_ap, in0=ta[0:rows, :], in1=tb[0:rows, :], op=ALU.add),
                     reads=[bta, btb], writes=out_bufs)

            def headnorm_rope(pi, gainb, out_ap, out_bufs):
                sq, bsq = t16()
                S.op(ACT, lambda: nc.scalar.activation(out=sq[:], in_=ps[pi][:], func=AF.Square), reads=[bps[pi]], writes=[bsq])
                pj = gp()
                S.op(PE, lambda: nc.tensor.matmul(ps[pj][:], ones_bf[:], sq[:], start=True, stop=True),
                     reads=[b_ones, bsq], writes=[bps[pj]])
                rs, brs = t32()
                S.op(ACT, lambda: nc.scalar.activation(out=rs[:], in_=ps[pj][:], func=AF.Sqrt, bias=epsr[:], scale=1.0 / 128),
                     reads=[bps[pj], b_epsr], writes=[brs])
                S.op(DVE, lambda: nc.vector.reciprocal(out=rs[:], in_=rs[:]), reads=[brs], writes=[brs])
                kn, bkn_ = t16()
                S.op(DVE, lambda: nc.vector.scalar_tensor_tensor(
                    out=kn[:], in0=ps[pi][:], scalar=gainb[0][:, 0:1], in1=rs[:], op0=ALU.mult, op1=ALU.mult),
                    reads=[bps[pi], gainb[1], brs], writes=[bkn_])
                rope_finish(pi, kn, bkn_, RB_bf, b_RB, tcB, b_tcB, 128, out_ap, out_bufs)

            def phase_a(job):
                nk = NK[job]
                for t in range(nk // TQ):
                    t0 = t * TQ
                    emit_casts(casts_per_tile)
                    load_x_tile(xk[job], t0, job)
                    load_tables(tAk[job], tBk[job], t0)
                    rms_lowrank(None, (akvn, b_akvn), C_CKV)

                    specs = []

                    def kr_consume(wt, bwt):
                        pi = gp()
                        proj(pi, wt, bwt, KC, 0, 64, lambda kc: hT[:, kc, :], [b_hT])
                        r16, br16 = t16()
                        S.op(ACT, lambda: nc.scalar.copy(out=r16[0:64, :], in_=ps[pi][0:64, :]), reads=[bps[pi]], writes=[br16])
                        o16, bo16 = t16()
                        rope_finish(pi, r16, br16, RA_bf, b_RA, tcA, b_tcA, 64, o16[0:64, :], [bo16])
                        S.dma(KR[job][:, t0:t0 + TQ], o16[0:64, :], reads=[bo16], writes=[bKV[job]])
                    specs.append((w_in_b, bw["w_in_b"], D, C_KR, 64, kr_consume))

                    def kb_consume(blk):
                        def f(wt, bwt):
                            for cc in range(2):
                                kvh = blk * 2 + cc
                                pi = gp()
                                proj(pi, wt, bwt, KC, cc * 128, 128, lambda kc: hT[:, kc, :], [b_hT])
                                o16, bo16 = t16()
                                headnorm_rope(pi, (bkn, b_bkn), o16[:], [bo16])
                                S.dma(KB[job][kvh, :, t0:t0 + TQ], o16[:], reads=[bo16], writes=[bKV[job]])
                        return f
                    for blk in range(2):
                        specs.append((w_in_b, bw["w_in_b"], D, C_KB + blk * 256, 256, kb_consume(blk)))

                    def vb_consume(blk):
                        def f(wt, bwt):
                            for half in range(2):
                                pi = gp()
                                for s2 in range(2):
                                    sub = half * 2 + s2
                                    for kc in range(KC):
                                        S.op(PE, lambda kc=kc, sub=sub, s2=s2: nc.tensor.matmul(
                                            ps[pi][:, s2 * 256:(s2 + 1) * 256], hT[:, kc, sub * 128:(sub + 1) * 128],
                                            wt[:, kc, 0:256], start=(kc == 0), stop=(kc == KC - 1)),
                                            reads=[bwt, b_hT], writes=[bps[pi]], signal=(kc == KC - 1 and s2 == 1))
                                o16, bo16 = t16()
                                S.op(ACT, lambda: nc.scalar.copy(out=o16[:], in_=ps[pi][:]), reads=[bps[pi]], writes=[bo16])
                                for s2 in range(2):
                                    sub = half * 2 + s2
                                    for hh in range(2):
                                        kvh = blk * 2 + hh
                                        S.dma(VB[job][kvh, t0 + sub * 128:t0 + (sub + 1) * 128, :],
                                              o16[:, s2 * 256 + hh * 128:s2 * 256 + (hh + 1) * 128], reads=[bo16], writes=[bKV[job]])
                        return f
                    for blk in range(2):
                        specs.append((w_in_b, bw["w_in_b"], D, C_VB + blk * 256, 256, vb_consume(blk)))

                    def ukv_consume(h):
                        def f(wt, bwt):
                            pi = gp()
                            proj(pi, wt, bwt, 4, 0, 128, lambda kc: cn[:, kc, :], [b_cn])
                            o16, bo16 = t16()
                            S.op(ACT, lambda: nc.scalar.copy(out=o16[:], in_=ps[pi][:]), reads=[bps[pi]], writes=[bo16])
                            S.dma(KA[job][h, :, t0:t0 + TQ], o16[:], reads=[bo16], writes=[bKV[job]])
                            pj = gp()
                            for sub in range(4):
                                for kc in range(4):
                                    S.op(PE, lambda kc=kc, sub=sub: nc.tensor.matmul(
                                        ps[pj][:, sub * 128:(sub + 1) * 128], cn[:, kc, sub * 128:(sub + 1) * 128],
                                        wt[:, kc, 128:256], start=(kc == 0), stop=(kc == 3)),
                                        reads=[bwt, b_cn], writes=[bps[pj]], signal=(kc == 3 and sub == 3))
                            v16, bv16 = t16()
                            S.op(DVE, lambda: nc.vector.tensor_copy(v16[:], ps[pj][:]), reads=[bps[pj]], writes=[bv16])
                            S.dma(VA[job][h, t0:t0 + TQ, :].rearrange("(s p) d -> p s d", p=128),
                                  v16[:].rearrange("p (s d) -> p s d", d=128), reads=[bv16], writes=[bKV[job]])
                        return f
                    for h in range(NH):
                        specs.append((w_ukv_b, bw["w_ukv_b"], 512, h * 256, 256, ukv_consume(h)))
                    run_blocks(specs)

            def attn_head(job, kparts, vsrc, q_aps, q_bufs, out_ap, out_bufs, scale, hidx):
                nk = NK[job]
                nch = nk // KCH
                po = 4 + (hidx % 2)
                pz = 6 + (hidx % 2)
                nblk = KCH // 128
                chunk_bufs = {}

                def load_chunk(c):
                    i = attn_state[0] % NKS_
                    attn_state[0] += 1
                    got = []
                    for (ksrc, rows, slots) in kparts:
                        t_, b_ = slots[i]
                        S.dma(t_[0:rows, :], ksrc[:, c * KCH:(c + 1) * KCH], reads=[bKV[job]], writes=[b_])
                        got.append((t_, b_, rows))
                    vt, bvt = vvs[i]
                    S.dma(vt[:], vsrc[c * KCH:(c + 1) * KCH, :].rearrange("(b p) d -> p b d", p=128), reads=[bKV[job]], writes=[bvt])
                    chunk_bufs[c] = (got, vt, bvt)

                items = [(c, kb) for c in range(nch) for kb in range(nblk)]
                n = len(items)
                sbank = {}

                def emit_s(i):
                    c, kb = items[i]
                    got, vt, bvt = chunk_bufs[c]
                    pi = gp()
                    sbank[i] = pi
                    np_ = len(got)
                    for j, (t_, b_, rows) in enumerate(got):
                        S.op(PE, lambda t_=t_, rows=rows, j=j: nc.tensor.matmul(
                            ps[pi][:], t_[0:rows, kb * 128:(kb + 1) * 128], q_aps[j], start=(j == 0), stop=(j == np_ - 1)),
                            reads=[b_] + q_bufs, writes=[bps[pi]], signal=(j == np_ - 1))

                load_chunk(0)
                if nch > 1:
                    load_chunk(1)
                LA = 2
                for i in range(min(LA, n)):
                    emit_s(i)
                for i in range(n):
                    c, kb = items[i]
                    if kb == 0 and c + 2 < nch:
                        load_chunk(c + 2)
                    if i + LA < n:
                        emit_s(i + LA)
                    pi = sbank.pop(i)
                    pt, bpt = pTs[i % 4]
                    S.op(ACT, lambda pt=pt, pi=pi: nc.scalar.activation(out=pt[:], in_=ps[pi][:], func=AF.Exp, scale=scale),
                         reads=[bps[pi]], writes=[bpt])
                    got, vt, bvt = chunk_bufs[c]
                    S.op(PE, lambda vt=vt, pt=pt, kb=kb: nc.tensor.matmul(
                        ps[po][:], vt[:, kb, :], pt[:], start=(i == 0), stop=(i == n - 1)),
                        reads=[bvt, bpt], writes=[bps[po]], signal=True)
                    a3 = i % 3
                    at, bat = paccs[hidx % 2][a3]
                    if a3 == 2:
                        if i < 3:
                            S.op(POOL, lambda at=at, pt=pt: nc.gpsimd.tensor_copy(at[:], pt[:]), reads=[bpt], writes=[bat])
                        else:
                            S.op(POOL, lambda at=at, pt=pt: nc.gpsimd.tensor_tensor(out=at[:], in0=pt[:], in1=at[:], op=ALU.add),
                                 reads=[bpt, bat], writes=[bat])
                    else:
                        if i < 3:
                            S.op(DVE, lambda at=at, pt=pt: nc.vector.tensor_copy(at[:], pt[:]), reads=[bpt], writes=[bat])
                        else:
                            S.op(DVE, lambda at=at, pt=pt: nc.vector.tensor_tensor(out=at[:], in0=pt[:], in1=at[:], op=ALU.add),
                                 reads=[bpt, bat], writes=[bat])
                nacc = min(3, n)
                for a3 in range(nacc):
                    at, bat = paccs[hidx % 2][a3]
                    S.op(PE, lambda at=at, a3=a3: nc.tensor.matmul(ps[pz][:], ones_f[:], at[:], start=(a3 == 0), stop=(a3 == nacc - 1)),
                         reads=[b_ones_f, bat], writes=[bps[pz]], signal=(a3 == nacc - 1))
                rs, brs = t32()
                S.op(DVE, lambda: nc.vector.reciprocal(out=rs[:], in_=ps[pz][:]), reads=[bps[pz]], writes=[brs])
                S.op(DVE, lambda: nc.vector.tensor_tensor(out=out_ap, in0=ps[po][:], in1=rs[:], op=ALU.mult),
                     reads=[bps[po], brs], writes=out_bufs)

            attn_state = [0]

            def layer_norm_inplace(X, bX, g_idx, b_idx):
                for dc in range(KC):
                    xb, bxb = t16()
                    S.op(ACT, lambda dc=dc, xb=xb: nc.scalar.copy(out=xb[:], in_=X[:, dc, :]), reads=[bX], writes=[bxb])
                    sq, bsq = t16()
                    S.op(ACT, lambda dc=dc, sq=sq: nc.scalar.activation(out=sq[:], in_=X[:, dc, :], func=AF.Square), reads=[bX], writes=[bsq])
                    S.op(PE, lambda dc=dc, xb=xb: nc.tensor.matmul(ps[4][:], onesD_bf[:], xb[:], start=(dc == 0), stop=(dc == KC - 1)),
                         reads=[b_onesD, bxb], writes=[bps[4]], signal=True)
                    S.op(PE, lambda dc=dc, sq=sq: nc.tensor.matmul(ps[5][:], onesD_bf[:], sq[:], start=(dc == 0), stop=(dc == KC - 1)),
                         reads=[b_onesD, bsq], writes=[bps[5]], signal=True)
                S.op(ACT, lambda: nc.scalar.copy(out=mean[:], in_=ps[4][:]), reads=[bps[4]], writes=[b_mean])
                m2, bm2 = t32()
                S.op(DVE, lambda: nc.vector.tensor_tensor(out=m2[:], in0=mean[:], in1=mean[:], op=ALU.mult), reads=[b_mean], writes=[bm2])
                S.op(DVE, lambda: nc.vector.tensor_tensor(out=rstd[:], in0=ps[5][:], in1=m2[:], op=ALU.subtract), reads=[bps[5], bm2], writes=[b_rstd])
                S.op(ACT, lambda: nc.scalar.activation(out=rstd[:], in_=rstd[:], func=AF.Sqrt, bias=epsl[:], scale=1.0),
                     reads=[b_rstd, b_epsl], writes=[b_rstd])
                S.op(DVE, lambda: nc.vector.reciprocal(out=rstd[:], in_=rstd[:]), reads=[b_rstd], writes=[b_rstd])
                for dc in range(KC):
                    S.op(POOL, lambda dc=dc: nc.gpsimd.tensor_tensor(out=X[:, dc, :], in0=X[:, dc, :], in1=mean[:], op=ALU.subtract),
                         reads=[bX, b_mean], writes=[bX])
                    S.op(DVE, lambda dc=dc: nc.vector.tensor_tensor(out=X[:, dc, :], in0=X[:, dc, :], in1=rstd[:], op=ALU.mult),
                         reads=[bX, b_rstd], writes=[bX])
                    S.op(DVE, lambda dc=dc: nc.vector.tensor_scalar(
                        out=X[:, dc, :], in0=X[:, dc, :], scalar1=lngb[:, g_idx, dc:dc + 1], scalar2=lngb[:, b_idx, dc:dc + 1],
                        op0=ALU.mult, op1=ALU.add), reads=[bX, b_lngb], writes=[bX])

            def phase_b(job, t, tok0):
                t0 = t * TQ
                load_x_tile(xq[job], t0, job)
                load_tables(tAq[job], tBq[job], t0)
                S.op(ACT, lambda: nc.scalar.mul(out=xT[:], in_=xT[:], mul=ALPHA), reads=[b_xT, b_hT], writes=[b_xT])
                rms_lowrank(None, (aqn, b_aqn), C_CQ)

                specs = []

                def uq_consume(h):
                    def f(wt, bwt):
                        pi = gp()
                        proj(pi, wt, bwt, 4, 0, 128, lambda kc: cn[:, kc, :], [b_cn])
                        S.op(ACT, lambda: nc.scalar.copy(out=qAn[:, h, :], in_=ps[pi][:]), reads=[bps[pi]], writes=[bq_heads["qAn"][h]])
                        pj = gp()
                        proj(pj, wt, bwt, 4, 128, 64, lambda kc: cn[:, kc, :], [b_cn])
                        r16, br16 = t16()
                        S.op(ACT, lambda: nc.scalar.copy(out=r16[0:64, :], in_=ps[pj][0:64, :]), reads=[bps[pj]], writes=[br16])
                        rope_finish(pj, r16, br16, RA_bf, b_RA, tcA, b_tcA, 64, qAr[0:64, h, :], [bq_heads["qAr"][h]])
                    return f
                for h in range(NH):
                    specs.append((w_uq_b, bw["w_uq_b"], 512, h * 192, 192, uq_consume(h)))

                def qb_consume(blk):
                    def f(wt, bwt):
                        for cc in range(2):
                            h = blk * 2 + cc
                            pi = gp()
                            proj(pi, wt, bwt, KC, cc * 128, 128, lambda kc: hT[:, kc, :], [b_hT])
                            headnorm_rope(pi, (bqn, b_bqn), qB[:, h, :], [bq_heads["qB"][h]])
                    return f
                for blk in range(8):
                    specs.append((w_in_b, bw["w_in_b"], D, C_QB + blk * 256, 256, qb_consume(blk)))
                run_blocks(specs)

                for h in range(NH):
                    attn_head(job, [(KA[job][h], 128, kts), (KR[job], 64, krs)], VA[job][h],
                              [qAn[:, h, :], qAr[0:64, h, :]], [bq_heads["qAn"][h], bq_heads["qAr"][h]],
                              qAn[:, h, :], [bq_heads["qAn"][h]], 192.0 ** -0.5, h)
                for h in range(NH):
                    kvh = h // 4
                    attn_head(job, [(KB[job][kvh], 128, kts)], VB[job][kvh],
                              [qB[:, h, :]], [bq_heads["qB"][h]], qB[:, h, :], [bq_heads["qB"][h]], 128.0 ** -0.5, h)

                mT, b_mT = qAr, b_qAr
                allA = bq_heads["qAn"] + bq_heads["qAr"]
                specs = []
                ta_keep = {}

                def o_consume(which, dcp):
                    src, bsrc_heads, base = (qAn, bq_heads["qAn"], 4) if which == 0 else (qB, bq_heads["qB"], 6)

                    def f(wt, bwt):
                        for cc in range(2):
                            proj(base + cc, wt, bwt, KC, cc * 128, 128, lambda kc: src[:, kc, :], bsrc_heads)
                    return f

                def g_consume(which, dcp):
                    base = 4 if which == 0 else 6

                    def f(wt, bwt):
                        for cc in range(2):
                            dc = dcp * 2 + cc
                            pi = gp()
                            proj(pi, wt, bwt, KC, cc * 128, 128, lambda kc: hT[:, kc, :], [b_hT])
                            sg, bsg = t32()
                            S.op(ACT, lambda: nc.scalar.activation(out=sg[:], in_=ps[pi][:], func=AF.Sigmoid), reads=[bps[pi]], writes=[bsg])
                            if which == 0:
                                ta, bta = t32()
                                S.op(DVE, lambda: nc.vector.tensor_tensor(out=ta[:], in0=ps[base + cc][:], in1=sg[:], op=ALU.mult),
                                     reads=[bps[base + cc], bsg], writes=[bta])
                                ta_keep[cc] = (ta, bta)
                            else:
                                ta, bta = ta_keep[cc]
                                tb, btb = t32()
                                S.op(DVE, lambda: nc.vector.tensor_tensor(out=tb[:], in0=ps[base + cc][:], in1=sg[:], op=ALU.mult),
                                     reads=[bps[base + cc], bsg], writes=[btb])
                                S.op(DVE, lambda: nc.vector.tensor_tensor(out=mT[:, dc, :], in0=ta[:], in1=tb[:], op=ALU.add),
                                     reads=[bta, btb], writes=[b_mT] + bq_heads["qAr"])
                    return f
                for dcp in range(8):
                    specs.append((w_ao_b, bw["w_ao_b"], D, dcp * 256, 256, o_consume(0, dcp)))
                    specs.append((w_in_b, bw["w_in_b"], D, C_GA + dcp * 256, 256, g_consume(0, dcp)))
                    specs.append((w_bo_b, bw["w_bo_b"], D, dcp * 256, 256, o_consume(1, dcp)))
                    specs.append((w_in_b, bw["w_in_b"], D, C_GB + dcp * 256, 256, g_consume(1, dcp)))

                def wout_consume(dcp):
                    def f(wt, bwt):
                        for cc in range(2):
                            dc = dcp * 2 + cc
                            pi = gp()
                            proj(pi, wt, bwt, KC, cc * 128, 128, lambda kc: mT[:, kc, :], [b_mT] + bq_heads["qAr"])
                            S.op(DVE, lambda: nc.vector.scalar_tensor_tensor(
                                out=xT[:, dc, :], in0=ps[pi][:], scalar=mod[:, G1 + dc, job:job + 1], in1=xT[:, dc, :],
                                op0=ALU.mult, op1=ALU.add), reads=[bps[pi], b_mod, b_xT], writes=[b_xT])
                    return f
                for dcp in range(8):
                    specs.append((w_out_b, bw["w_out_b"], D, dcp * 256, 256, wout_consume(dcp)))
                run_blocks(specs)

                layer_norm_inplace(xT, b_xT, 0, 1)
                S.dma(x1n_s[:, tok0:tok0 + TQ].rearrange("(kc p) n -> p kc n", p=128), xT[:], reads=[b_xT], writes=[b_x1n_s])

                for dc in range(KC):
                    h2f, bh2f = t32()
                    S.op(DVE, lambda dc=dc, h2f=h2f: nc.vector.tensor_scalar(
                        out=h2f[:], in0=xT[:, dc, :], scalar1=mod[:, SC2 + dc, job:job + 1], scalar2=mod[:, SH2 + dc, job:job + 1],
                        op0=ALU.mult, op1=ALU.add), reads=[b_xT, b_mod], writes=[bh2f])
                    S.op(PE, lambda dc=dc, h2f=h2f: nc.tensor.matmul(ps[6][0:72, :], w_r[:, dc, :], h2f[:], start=(dc == 0), stop=(dc == KC - 1)),
                         reads=[b_w_r, bh2f], writes=[bps[6]], signal=True)
                    S.op(ACT, lambda dc=dc, h2f=h2f: nc.scalar.copy(out=hT[:, dc, :], in_=h2f[:]), reads=[bh2f], writes=[b_hT])
                S.dma(h2_s[:, tok0:tok0 + TQ].rearrange("(kc p) n -> p kc n", p=128), hT[:], reads=[b_hT], writes=[b_h2_s])
                lt, blt = t32()
                S.op(ACT, lambda: nc.scalar.copy(out=lt[0:72, :], in_=ps[6][0:72, :]), reads=[bps[6]], writes=[blt])
                pc = 7
                for sub in range(4):
                    pi = gp()
                    S.op(PE, lambda: nc.tensor.transpose(out=ps[pi][:, 0:72], in_=lt[0:72, sub * 128:(sub + 1) * 128], identity=ident[0:72, 0:72]),
                         reads=[blt, b_ident], writes=[bps[pi]])
                    S.op(DVE, lambda: nc.vector.tensor_tensor(out=lg[:], in0=ps[pi][:, 0:72], in1=b_r[:], op=ALU.add),
                         reads=[bps[pi], b_b_r], writes=[b_lg])
                    (gmax, b_gmax), (ngmax, b_ngmax), (gsum, b_gsum), (pgrp, b_pgrp), (m1, b_m1), (m2_, b_m2), \
                        (dd, b_dd), (w1, b_w1), (w2, b_w2), (den, b_den) = sc
                    (ohg, b_ohg), (eg_, b_eg), (ein, b_ein), (oh1, b_oh1), (e2, b_e2), (oh2, b_oh2), (cwe, b_cwe), (junk, b_junk) = sm
                    S.op(DVE, lambda: nc.vector.tensor_reduce(out=gmax[:], in_=lg[:, 0:8], axis=AX.X, op=ALU.max), reads=[b_lg], writes=[b_gmax])
                    S.op(DVE, lambda: nc.vector.tensor_scalar(out=ohg[:], in0=lg[:, 0:8], scalar1=gmax[:, 0:1], scalar2=None, op0=ALU.is_equal),
                         reads=[b_lg, b_gmax], writes=[b_ohg])
                    S.op(DVE, lambda: nc.vector.tensor_scalar(out=ngmax[:], in0=gmax[:], scalar1=-1.0, scalar2=None, op0=ALU.mult),
                         reads=[b_gmax], writes=[b_ngmax])
                    S.op(DVE, lambda: nc.vector.memset(gsum[:], 0.0), writes=[b_gsum])
                    S.op(ACT, lambda: nc.scalar.activation(out=eg_[:], in_=lg[:, 0:8], func=AF.Exp, bias=ngmax[:], scale=1.0, accum_out=gsum[:]),
                         reads=[b_lg, b_ngmax], writes=[b_eg, b_gsum])
                    S.op(DVE, lambda: nc.vector.reciprocal(out=pgrp[:], in_=gsum[:]), reads=[b_gsum], writes=[b_pgrp])
                    for g in range(8):
                        if g == 0:
                            S.op(DVE, lambda: nc.vector.tensor_scalar(out=ein[:], in0=lg[:, 8:16], scalar1=ohg[:, 0:1], scalar2=None, op0=ALU.mult),
                                 reads=[b_lg, b_ohg], writes=[b_ein])
                        else:
                            S.op(DVE, lambda g=g: nc.vector.scalar_tensor_tensor(
                                out=ein[:], in0=lg[:, 8 + g * 8:16 + g * 8], scalar=ohg[:, g:g + 1], in1=ein[:], op0=ALU.mult, op1=ALU.add),
                                reads=[b_lg, b_ohg, b_ein], writes=[b_ein])
                    S.op(DVE, lambda: nc.vector.tensor_reduce(out=m1[:], in_=ein[:], axis=AX.X, op=ALU.max), reads=[b_ein], writes=[b_m1])
                    S.op(DVE, lambda: nc.vector.tensor_scalar(out=oh1[:], in0=ein[:], scalar1=m1[:, 0:1], scalar2=None, op0=ALU.is_equal),
                         reads=[b_ein, b_m1], writes=[b_oh1])
                    S.op(DVE, lambda: nc.vector.scalar_tensor_tensor(out=e2[:], in0=oh1[:], scalar=-1e30, in1=ein[:], op0=ALU.mult, op1=ALU.add),
                         reads=[b_oh1, b_ein], writes=[b_e2])
                    S.op(DVE, lambda: nc.vector.tensor_reduce(out=m2_[:], in_=e2[:], axis=AX.X, op=ALU.max), reads=[b_e2], writes=[b_m2])
                    S.op(DVE, lambda: nc.vector.tensor_scalar(out=oh2[:], in0=e2[:], scalar1=m2_[:, 0:1], scalar2=None, op0=ALU.is_equal),
                         reads=[b_e2, b_m2], writes=[b_oh2])
                    S.op(DVE, lambda: nc.vector.tensor_tensor(out=dd[:], in0=m2_[:], in1=m1[:], op=ALU.subtract), reads=[b_m1, b_m2], writes=[b_dd])
                    S.op(ACT, lambda: nc.scalar.activation(out=dd[:], in_=dd[:], func=AF.Exp), reads=[b_dd], writes=[b_dd])
                    S.op(DVE, lambda: nc.vector.tensor_scalar(out=den[:], in0=dd[:], scalar1=1.0, scalar2=None, op0=ALU.add), reads=[b_dd], writes=[b_den])
                    S.op(DVE, lambda: nc.vector.reciprocal(out=w1[:], in_=den[:]), reads=[b_den], writes=[b_w1])
                    S.op(DVE, lambda: nc.vector.tensor_tensor(out=w2[:], in0=dd[:], in1=w1[:], op=ALU.mult), reads=[b_dd, b_w1], writes=[b_w2])
                    S.op(DVE, lambda: nc.vector.tensor_tensor(out=w1[:], in0=w1[:], in1=pgrp[:], op=ALU.mult), reads=[b_w1, b_pgrp], writes=[b_w1])
                    S.op(DVE, lambda: nc.vector.tensor_tensor(out=w2[:], in0=w2[:], in1=pgrp[:], op=ALU.mult), reads=[b_w2, b_pgrp], writes=[b_w2])
                    S.op(DVE, lambda: nc.vector.tensor_scalar(out=cwe[:], in0=oh1[:], scalar1=w1[:, 0:1], scalar2=None, op0=ALU.mult),
                         reads=[b_oh1, b_w1], writes=[b_cwe])
                    S.op(DVE, lambda: nc.vector.scalar_tensor_tensor(out=cwe[:], in0=oh2[:], scalar=w2[:, 0:1], in1=cwe[:], op0=ALU.mult, op1=ALU.add),
                         reads=[b_oh2, b_w2, b_cwe], writes=[b_cwe])
                    for g in range(8):
                        S.op(DVE, lambda g=g: nc.vector.tensor_scalar(out=cw[:, g * 8:(g + 1) * 8], in0=cwe[:], scalar1=ohg[:, g:g + 1], scalar2=None, op0=ALU.mult),
                             reads=[b_cwe, b_ohg], writes=[b_cw])
                    S.op(PE, lambda: nc.tensor.transpose(out=ps[pc][0:64, sub * 128:(sub + 1) * 128], in_=cw[:], identity=ident[:]),
                         reads=[b_cw, b_ident], writes=[bps[pc]])
                S.op(ACT, lambda: nc.scalar.copy(out=cwT[:], in_=ps[pc][0:64, :]), reads=[bps[pc]], writes=[b_cwT])
                S.dma(cw_s[:, tok0:tok0 + TQ], cwT[:], reads=[b_cwT], writes=[b_cw_s])

            tok0 = 0
            tiles = []
            for job in range(2):
                phase_a(job)
            emit_casts(len(cast_list))
            for job in range(2):
                for t in range(NQ[job] // TQ):
                    phase_b(job, t, tok0)
                    tiles.append((job, t, tok0))
                    tok0 += TQ
        S.barrier()

        with ExitStack() as es2:
            h2m, b_h2m = sb(es2, "h2m", [128, KC, TQ], BF16)
            acc, b_acc = sb(es2, "acc", [128, KC, TQ], F32)
            wg = [sb(es2, f"wg{i}", [128, KC, DE], BF16) for i in range(2)]
            wu = [sb(es2, f"wu{i}", [128, KC, DE], BF16) for i in range(2)]
            wd = [sb(es2, f"wd{i}", [128, 4, D], BF16) for i in range(2)]
            hid, b_hid = sb(es2, "hid", [128, 4, TQ], BF16)
            cwb = [sb(es2, f"cwb{i}", [128, TQ], F32) for i in range(3)]
            sgs = [sb(es2, f"sgs{i}", [128, TQ], F32) for i in range(3)]
            xa = [sb(es2, f"xa{i}", [128, TQ], F32) for i in range(2)]
            rstd2, b_rstd2 = sb(es2, "rstd2", [128, TQ], F32)
            mean2, b_mean2 = sb(es2, "mean2", [128, TQ], F32)
            m2t, b_m2t = sb(es2, "m2t", [128, TQ], F32)
            l16 = [sb(es2, f"l16_{i}", [128, TQ], BF16) for i in range(4)]

            hid2, b_hid2 = sb(es2, "hid2", [128, 4, TQ], BF16)
            hids = [(hid, b_hid), (hid2, b_hid2)]

            def load_gu(e, tok0, k):
                i = k % 2
                S.dma(wg[i][0][:], eg_b[e].rearrange("(kc p) n -> p kc n", p=128), reads=[bw["e_b"]], writes=[wg[i][1]])
                S.dma(wu[i][0][:], eu_b[e].rearrange("(kc p) n -> p kc n", p=128), reads=[bw["e_b"]], writes=[wu[i][1]])
                S.dma(cwb[k % 3][0][:], cw_s[e:e + 1, tok0:tok0 + TQ].partition_broadcast(128), reads=[b_cw_s], writes=[cwb[k % 3][1]])

            def load_d(e, k):
                i = k % 2
                S.dma(wd[i][0][:], ed_b[e].rearrange("(kc p) n -> p kc n", p=128), reads=[bw["e_b"]], writes=[wd[i][1]])

            def gate_up(k):
                i = k % 2
                wgt, bwg = wg[i]
                wut, bwu = wu[i]
                cwt_, bcw = cwb[k % 3]
                hd, bhd = hids[i]
                for hc in range(4):
                    pa = gp()
                    for kc in range(KC):
                        S.op(PE, lambda kc=kc: nc.tensor.matmul(ps[pa][:], wgt[:, kc, hc * 128:(hc + 1) * 128], h2m[:, kc, :],
                                                               start=(kc == 0), stop=(kc == KC - 1)),
                             reads=[bwg, b_h2m], writes=[bps[pa]], signal=(kc == KC - 1))
                    pb = gp()
                    for kc in range(KC):
                        S.op(PE, lambda kc=kc: nc.tensor.matmul(ps[pb][:], wut[:, kc, hc * 128:(hc + 1) * 128], h2m[:, kc, :],
                                                               start=(kc == 0), stop=(kc == KC - 1)),
                             reads=[bwu, b_h2m], writes=[bps[pb]], signal=(kc == KC - 1))
                    sg, bsg = sgs[(k * 4 + hc) % 3]
                    S.op(ACT, lambda: nc.scalar.activation(out=sg[:], in_=ps[pa][:], func=AF.Silu), reads=[bps[pa]], writes=[bsg])
                    S.op(POOL, lambda: nc.gpsimd.tensor_tensor(out=sg[:], in0=sg[:], in1=cwt_[:], op=ALU.mult), reads=[bsg, bcw], writes=[bsg])
                    S.op(DVE, lambda: nc.vector.tensor_tensor(out=hd[:, hc, :], in0=ps[pb][:], in1=sg[:], op=ALU.mult),
                         reads=[bps[pb], bsg], writes=[bhd])

            def down(k, first):
                i = k % 2
                wdt, bwd = wd[i]
                hd, bhd = hids[i]
                for dc in range(KC):
                    pd = 4 + (dc % 4)
                    for kc in range(4):
                        S.op(PE, lambda kc=kc: nc.tensor.matmul(ps[pd][:], wdt[:, kc, dc * 128:(dc + 1) * 128], hd[:, kc, :],
                                                               start=(kc == 0), stop=(kc == 3)),
                             reads=[bwd, bhd], writes=[bps[pd]], signal=(kc == 3))
                    if first:
                        S.op(DVE, lambda: nc.vector.tensor_copy(acc[:, dc, :], ps[pd][:]), reads=[bps[pd]], writes=[b_acc])
                    else:
                        S.op(DVE, lambda: nc.vector.tensor_tensor(out=acc[:, dc, :], in0=ps[pd][:], in1=acc[:, dc, :], op=ALU.add),
                             reads=[bps[pd], b_acc], writes=[b_acc])

            k = 0
            for (job, t, tok0) in tiles:
                S.dma(h2m[:], h2_s[:, tok0:tok0 + TQ].rearrange("(kc p) n -> p kc n", p=128), reads=[b_h2_s], writes=[b_h2m])
                load_gu(0, tok0, k)
                load_d(0, k)
                for e in range(NE):
                    if e + 1 < NE:
                        load_gu(e + 1, tok0, k + 1)
                    gate_up(k)
                    if e > 0:
                        down(k - 1, e - 1 == 0)
                    if e + 1 < NE:
                        load_d(e + 1, k + 1)
                    k += 1
                down(k - 1, False)
                for dc in range(KC):
                    xt_, bxt = xa[dc % 2]
                    S.dma(xt_[:], x1n_s[dc * 128:(dc + 1) * 128, tok0:tok0 + TQ], reads=[b_x1n_s], writes=[bxt])
                    S.op(DVE, lambda: nc.vector.tensor_scalar(out=acc[:, dc, :], in0=acc[:, dc, :], scalar1=mod[:, G2 + dc, job:job + 1],
                                                              scalar2=None, op0=ALU.mult), reads=[b_acc, b_mod], writes=[b_acc])
                    S.op(DVE, lambda: nc.vector.scalar_tensor_tensor(out=acc[:, dc, :], in0=xt_[:], scalar=ALPHA, in1=acc[:, dc, :],
                                                                     op0=ALU.mult, op1=ALU.add), reads=[bxt, b_acc], writes=[b_acc])
                for dc in range(KC):
                    xb, bxb = l16[(2 * dc) % 4]
                    sq, bsq = l16[(2 * dc + 1) % 4]
                    S.op(ACT, lambda: nc.scalar.copy(out=xb[:], in_=acc[:, dc, :]), reads=[b_acc], writes=[bxb])
                    S.op(ACT, lambda: nc.scalar.activation(out=sq[:], in_=acc[:, dc, :], func=AF.Square), reads=[b_acc], writes=[bsq])
                    S.op(PE, lambda: nc.tensor.matmul(ps[0][:], onesD_bf[:], xb[:], start=(dc == 0), stop=(dc == KC - 1)),
                         reads=[b_onesD, bxb], writes=[bps[0]], signal=True)
                    S.op(PE, lambda: nc.tensor.matmul(ps[1][:], onesD_bf[:], sq[:], start=(dc == 0), stop=(dc == KC - 1)),
                         reads=[b_onesD, bsq], writes=[bps[1]], signal=True)
                S.op(ACT, lambda: nc.scalar.copy(out=mean2[:], in_=ps[0][:]), reads=[bps[0]], writes=[b_mean2])
                S.op(DVE, lambda: nc.vector.tensor_tensor(out=m2t[:], in0=mean2[:], in1=mean2[:], op=ALU.mult), reads=[b_mean2], writes=[b_m2t])
                S.op(DVE, lambda: nc.vector.tensor_tensor(out=rstd2[:], in0=ps[1][:], in1=m2t[:], op=ALU.subtract), reads=[bps[1], b_m2t], writes=[b_rstd2])
                S.op(ACT, lambda: nc.scalar.activation(out=rstd2[:], in_=rstd2[:], func=AF.Sqrt, bias=epsl[:], scale=1.0),
                     reads=[b_rstd2, b_epsl], writes=[b_rstd2])
                S.op(DVE, lambda: nc.vector.reciprocal(out=rstd2[:], in_=rstd2[:]), reads=[b_rstd2], writes=[b_rstd2])
                for dc in range(KC):
                    S.op(POOL, lambda: nc.gpsimd.tensor_tensor(out=acc[:, dc, :], in0=acc[:, dc, :], in1=mean2[:], op=ALU.subtract),
                         reads=[b_acc, b_mean2], writes=[b_acc])
                    S.op(DVE, lambda: nc.vector.tensor_tensor(out=acc[:, dc, :], in0=acc[:, dc, :], in1=rstd2[:], op=ALU.mult),
                         reads=[b_acc, b_rstd2], writes=[b_acc])
                    S.op(DVE, lambda: nc.vector.tensor_scalar(out=acc[:, dc, :], in0=acc[:, dc, :], scalar1=lngb[:, 2, dc:dc + 1],
                                                              scalar2=lngb[:, 3, dc:dc + 1], op0=ALU.mult, op1=ALU.add),
                         reads=[b_acc, b_lngb], writes=[b_acc])
                b_y = Buf("y")
                S.dma(y_d[job][:, t * TQ:(t + 1) * TQ].rearrange("(kc p) n -> p kc n", p=128), acc[:], reads=[b_acc], writes=[b_y])
                S._waits(S.SP, [b_y], [])
        S.barrier()
    return nc


def _rope_tables(S):
    inv = (THETA ** (-np.arange(0, 64, 2, dtype=np.float32) / np.float32(64))).astype(np.float32)
    t = np.arange(S, dtype=np.int32)
    row = (t // 64).astype(np.float32)
    col = (t % 64).astype(np.float32)
    ang_a = t.astype(np.float32)[:, None] * inv[None, :]
    ang_r = row[:, None] * inv[None, :]
    ang_c = col[:, None] * inv[None, :]
    tA = np.empty((2, 64, S), np.float32)
    tB = np.empty((2, 128, S), np.float32)
    for k, fn in enumerate((np.cos, np.sin)):
        a = fn(ang_a).astype(np.float32).T
        tA[k, 0:32] = a
        tA[k, 32:64] = a
        r = fn(ang_r).astype(np.float32).T
        c = fn(ang_c).astype(np.float32).T
        tB[k, 0:32] = r
        tB[k, 32:64] = r
        tB[k, 64:96] = c
        tB[k, 96:128] = c
    return tA, tB


def _fm(v, kc):
    return np.ascontiguousarray(np.asarray(v, np.float32).reshape(kc, 128).T)


def run(inputs, n_prompt, s_prompt, s_sample):
    f = lambda k: np.asarray(inputs[k], np.float32)
    xp, xs = f("x_prompt"), f("x_sample")
    NKP, NKS = s_prompt, s_sample
    NQP, NQS = s_prompt // 2, s_sample // 8
    nc = build(NKP, NQP, NKS, NQS)

    tAp, tBp = _rope_tables(NKP)
    tAs, tBs = _rope_tables(NKS)
    RA = np.zeros((64, 64), np.float32)
    for i in range(32):
        RA[i + 32, i] = -1.0
        RA[i, i + 32] = 1.0
    RB = np.zeros((128, 128), np.float32)
    RB[0:64, 0:64] = RA
    RB[64:128, 64:128] = RA
    shared = {
        "RA": RA, "RB": RB, "ident": np.eye(128, dtype=np.float32),
        "w_ada": f("w_ada")[0], "b_adaT": _fm(f("b_ada")[0], 96),
        "w_in": f("w_in")[0], "a_q_norm": _fm(f("a_q_norm")[0], 4), "a_kv_norm": _fm(f("a_kv_norm")[0], 4),
        "a_w_uq": f("a_w_uq")[0], "a_w_ukv": f("a_w_ukv")[0], "a_w_o": f("a_w_o")[0],
        "b_q_norm": f("b_q_norm")[0].reshape(128, 1), "b_k_norm": f("b_k_norm")[0].reshape(128, 1),
        "b_w_o": f("b_w_o")[0], "w_out": f("w_out")[0],
        "ln_gb": np.ascontiguousarray(np.stack([_fm(f("ln1_g")[0], 16), _fm(f("ln1_b")[0], 16),
                                                _fm(f("ln2_g")[0], 16), _fm(f("ln2_b")[0], 16)], axis=1)),
        "w_r": np.ascontiguousarray(np.concatenate([f("w_group")[0], f("w_expert")[0]], axis=1)),
        "b_r": np.ascontiguousarray(np.broadcast_to(np.concatenate([f("b_group")[0], f("b_expert")[0]])[None, :], (128, 72))),
        "e_w_gate": f("e_w_gate")[0], "e_w_up": f("e_w_up")[0], "e_w_down": f("e_w_down")[0],
    }
    xpT = [np.ascontiguousarray(xp[b].T) for b in range(n_prompt)]
    xsT = np.ascontiguousarray(xs[0].T)
    cp, cs_ = f("c_prompt"), f("c_sample")
    in_maps = []
    for c in range(8):
        b, half = c // 2, c % 2
        b = min(b, n_prompt - 1)
        cT = np.stack([cp[b], cs_[0]], axis=1)
        cT = np.ascontiguousarray(cT.reshape(KC, 128, 2).transpose(1, 0, 2))
        m = dict(shared)
        m.update({
            "xkp": xpT[b], "xks": xsT,
            "xqp": np.ascontiguousarray(xpT[b][:, half * NQP:(half + 1) * NQP]),
            "xqs": np.ascontiguousarray(xsT[:, c * NQS:(c + 1) * NQS]),
            "tAkp": tAp, "tBkp": tBp, "tAks": tAs, "tBks": tBs,
            "tAqp": np.ascontiguousarray(tAp[:, :, half * NQP:(half + 1) * NQP]),
            "tBqp": np.ascontiguousarray(tBp[:, :, half * NQP:(half + 1) * NQP]),
            "tAqs": np.ascontiguousarray(tAs[:, :, c * NQS:(c + 1) * NQS]),
            "tBqs": np.ascontiguousarray(tBs[:, :, c * NQS:(c + 1) * NQS]),
            "cT": cT,
        })
        in_maps.append(m)
    res = run_bass_kernel_spmd(nc, in_maps, core_ids=list(range(8)))
    y_p = np.empty((n_prompt, s_prompt, D), np.float32)
    y_s = np.empty((1, s_sample, D), np.float32)
    for c in range(8):
        b, half = c // 2, c % 2
        r = res.results[c]
        if b < n_prompt:
            y_p[b, half * NQP:(half + 1) * NQP, :] = np.asarray(r["yp"]).T
        y_s[0, c * NQS:(c + 1) * NQS, :] = np.asarray(r["ys"]).T
    return y_p, y_s


def kernel(**inputs):
    return run(inputs, 4, 8192, 16384)
```

```python
import numpy as np
from contextlib import ExitStack
import concourse.bass as bass
import concourse.mybir as mybir
from concourse.bass_utils import run_bass_kernel_spmd

F32 = mybir.dt.float32
BF16 = mybir.dt.bfloat16
AF = mybir.ActivationFunctionType
ALU = mybir.AluOpType
AX = mybir.AxisListType

D = 2048
KC = 16
NH = 16
NKVH = 4
NE = 64
DE = 512
TQ = 512
ALPHA = 2.0 ** 0.25
RMS_EPS = 1e-6
LN_EPS = 1e-5
THETA = 10000.0
C_CQ, C_CKV, C_KR, C_QB, C_KB, C_VB, C_GA, C_GB = 0, 512, 1024, 1088, 3136, 3648, 4160, 6208
D_IN = 8256


class Tok:
    def __init__(self, sem, name):
        self.sem = sem
        self.name = name
        self.count = 0


class Eng(Tok):
    def __init__(self, eng, sem, name, strict_self):
        super().__init__(sem, name)
        self.eng = eng
        self.waited = {}
        self.strict_self = strict_self


class Buf:
    __slots__ = ("name", "w", "r", "multi")

    def __init__(self, name, multi=False):
        self.name = name
        self.w = {}
        self.r = {}
        self.multi = multi


class Sched:
    def __init__(self, nc, es, n_dma_sems=24):
        self.nc = nc

        def mk(eng, name, strict):
            return Eng(eng, es.enter_context(nc.semaphore("s_" + name)), name, strict)

        self.PE = mk(nc.tensor, "pe", False)
        self.ACT = mk(nc.scalar, "act", True)
        self.DVE = mk(nc.vector, "dve", True)
        self.POOL = mk(nc.gpsimd, "pool", True)
        self.SP = mk(nc.sync, "sp", False)
        self.engs = [self.PE, self.ACT, self.DVE, self.POOL, self.SP]
        self.dma_toks = {}
        for q in ("sp", "pool"):
            self.dma_toks[q] = [Tok(es.enter_context(nc.semaphore(f"d_{q}{i}")), f"d_{q}{i}")
                                for i in range(n_dma_sems)]
        self.dma_rr = {"sp": 0, "pool": 0}
        self.n_inst = 0

    def _waits(self, E, reads, writes):
        need = {}
        for b in reads:
            for t, v in b.w.items():
                if need.get(t, 0) < v:
                    need[t] = v
        for b in writes:
            if b.multi:
                continue
            for t, v in b.w.items():
                if need.get(t, 0) < v:
                    need[t] = v
            for t, v in b.r.items():
                if need.get(t, 0) < v:
                    need[t] = v
        for t, v in need.items():
            if t is E and not E.strict_self:
                continue
            if E.waited.get(t, 0) >= v:
                continue
            E.eng.wait_ge(t.sem, v)
            E.waited[t] = v
            self.n_inst += 1

    def _mark(self, tok, mark, reads, writes):
        for b in reads:
            if b.r.get(tok, 0) < mark:
                b.r[tok] = mark
        for b in writes:
            if b.multi:
                if b.w.get(tok, 0) < mark:
                    b.w[tok] = mark
            else:
                b.w = {tok: mark}
                b.r = {}

    def op(self, E, fn, reads=(), writes=(), signal=True):
        self._waits(E, reads, writes)
        ins = fn()
        self.n_inst += 1
        if signal:
            E.count += 1
            ins.then_inc(E.sem, 1)
            mark = E.count
        else:
            mark = E.count + 1
        self._mark(E, mark, reads, writes)
        return ins

    def dma(self, out_ap, in_ap, reads=(), writes=(), q="sp"):
        E = self.SP if q == "sp" else self.POOL
        toks = self.dma_toks[q]
        t = toks[self.dma_rr[q] % len(toks)]
        self.dma_rr[q] += 1
        if t.count > 0 and E.waited.get(t, 0) < t.count:
            E.eng.wait_ge(t.sem, t.count)
            E.waited[t] = t.count
        self._waits(E, reads, writes)
        ins = E.eng.dma_start(out=out_ap, in_=in_ap)
        t.count += 16
        ins.then_inc(t.sem, 16)
        self.n_inst += 1
        self._mark(t, t.count, reads, writes)
        return ins

    def barrier(self):
        toks = list(self.engs)
        for q in self.dma_toks.values():
            toks += q
        for E in self.engs:
            for t in toks:
                if t is E or t.count == 0 or E.waited.get(t, 0) >= t.count:
                    continue
                E.eng.wait_ge(t.sem, t.count)
                E.waited[t] = t.count


def build(NKP, NQP, NKS, NQS):
    T = NQP + NQS
    nc = bass.Bass("TRN2", target_bir_lowering=False)

    def din(name, shape, dt=F32):
        return nc.dram_tensor(name, list(shape), dt, kind="ExternalInput").ap()

    def dscr(name, shape, dt):
        return nc.dram_tensor(name, list(shape), dt).ap()

    NK = [NKP, NKS]
    NQ = [NQP, NQS]
    xk = [din("xkp", [D, NKP]), din("xks", [D, NKS])]
    xq = [din("xqp", [D, NQP]), din("xqs", [D, NQS])]
    tAk = [din("tAkp", [2, 64, NKP]), din("tAks", [2, 64, NKS])]
    tBk = [din("tBkp", [2, 128, NKP]), din("tBks", [2, 128, NKS])]
    tAq = [din("tAqp", [2, 64, NQP]), din("tAqs", [2, 64, NQS])]
    tBq = [din("tBqp", [2, 128, NQP]), din("tBqs", [2, 128, NQS])]
    cT_d = din("cT", [128, KC, 2])
    RA_d = din("RA", [64, 64])
    RB_d = din("RB", [128, 128])
    ident_d = din("ident", [128, 128])
    w_ada_d = din("w_ada", [D, 6 * D])
    b_ada_d = din("b_adaT", [128, 96])
    w_in_d = din("w_in", [D, D_IN])
    aqn_d = din("a_q_norm", [128, 4])
    akvn_d = din("a_kv_norm", [128, 4])
    w_uq_d = din("a_w_uq", [512, 3072])
    w_ukv_d = din("a_w_ukv", [512, 4096])
    w_ao_d = din("a_w_o", [D, D])
    bqn_d = din("b_q_norm", [128, 1])
    bkn_d = din("b_k_norm", [128, 1])
    w_bo_d = din("b_w_o", [D, D])
    w_out_d = din("w_out", [D, D])
    ln_d = din("ln_gb", [128, 4, KC])
    w_r_d = din("w_r", [D, 72])
    b_r_d = din("b_r", [128, 72])
    eg_d = din("e_w_gate", [NE, D, DE])
    eu_d = din("e_w_up", [NE, D, DE])
    ed_d = din("e_w_down", [NE, DE, D])
    y_d = [nc.dram_tensor("yp", [D, NQP], F32, kind="ExternalOutput").ap(),
           nc.dram_tensor("ys", [D, NQS], F32, kind="ExternalOutput").ap()]

    w_in_b = dscr("w_in_b", [D, D_IN], BF16)
    w_uq_b = dscr("w_uq_b", [512, 3072], BF16)
    w_ukv_b = dscr("w_ukv_b", [512, 4096], BF16)
    w_ao_b = dscr("w_ao_b", [D, D], BF16)
    w_bo_b = dscr("w_bo_b", [D, D], BF16)
    w_out_b = dscr("w_out_b", [D, D], BF16)
    eg_b = dscr("eg_b", [NE, D, DE], BF16)
    eu_b = dscr("eu_b", [NE, D, DE], BF16)
    ed_b = dscr("ed_b", [NE, DE, D], BF16)
    KA = [dscr(f"KA{j}", [NH, 128, NK[j]], BF16) for j in range(2)]
    KR = [dscr(f"KR{j}", [64, NK[j]], BF16) for j in range(2)]
    VA = [dscr(f"VA{j}", [NH, NK[j], 128], BF16) for j in range(2)]
    KB = [dscr(f"KB{j}", [NKVH, 128, NK[j]], BF16) for j in range(2)]
    VB = [dscr(f"VB{j}", [NKVH, NK[j], 128], BF16) for j in range(2)]
    x1n_s = dscr("x1n_s", [D, T], F32)
    h2_s = dscr("h2_s", [D, T], BF16)
    cw_s = dscr("cw_s", [NE, T], F32)

    with ExitStack() as es:
        S = Sched(nc, es)
        PE, ACT, DVE, POOL = S.PE, S.ACT, S.DVE, S.POOL

        def sb(stack, name, shape, dt):
            return stack.enter_context(nc.sbuf_tensor("sb_" + name, list(shape), dt)), Buf(name)

        ps = [es.enter_context(nc.psum_tensor(f"ps{i}", [128, 512], F32)) for i in range(8)]
        bps = [Buf(f"ps{i}") for i in range(8)]
        gp_state = [0]

        def gp():
            i = gp_state[0] % 4
            gp_state[0] += 1
            return i

        ones_bf, b_ones = sb(es, "ones_bf", [128, 128], BF16)
        onesD_bf, b_onesD = sb(es, "onesD_bf", [128, 128], BF16)
        RA_bf, b_RA = sb(es, "RA_bf", [64, 64], BF16)
        RB_bf, b_RB = sb(es, "RB_bf", [128, 128], BF16)
        ident, b_ident = sb(es, "ident", [128, 128], F32)
        epsr, b_epsr = sb(es, "epsr", [128, 1], F32)
        epsl, b_epsl = sb(es, "epsl", [128, 1], F32)
        mod, b_mod = sb(es, "mod", [128, 96, 2], F32)
        aqn, b_aqn = sb(es, "aqn", [128, 4], F32)
        akvn, b_akvn = sb(es, "akvn", [128, 4], F32)
        bqn, b_bqn = sb(es, "bqn", [128, 1], F32)
        bkn, b_bkn = sb(es, "bkn", [128, 1], F32)
        lngb, b_lngb = sb(es, "lngb", [128, 4, KC], F32)
        w_r, b_w_r = sb(es, "w_r", [128, KC, 72], F32)
        b_r, b_b_r = sb(es, "b_r", [128, 72], F32)
        stage, b_stage = sb(es, "stage", [128, 128], F32)
        ones_f, b_ones_f = sb(es, "ones_f", [128, 128], F32)

        S.op(DVE, lambda: nc.vector.memset(ones_bf[:], 1.0), writes=[b_ones])
        S.op(DVE, lambda: nc.vector.memset(ones_f[:], 1.0), writes=[b_ones_f])
        S.op(DVE, lambda: nc.vector.memset(onesD_bf[:], 1.0 / D), writes=[b_onesD])
        S.op(DVE, lambda: nc.vector.memset(epsr[:], RMS_EPS), writes=[b_epsr])
        S.op(DVE, lambda: nc.vector.memset(epsl[:], LN_EPS), writes=[b_epsl])
        S.dma(stage[:], RB_d, writes=[b_stage])
        S.op(DVE, lambda: nc.vector.tensor_copy(RB_bf[:], stage[:]), reads=[b_stage], writes=[b_RB])
        S.dma(stage[0:64, 0:64], RA_d, writes=[b_stage])
        S.op(DVE, lambda: nc.vector.tensor_copy(RA_bf[:], stage[0:64, 0:64]), reads=[b_stage], writes=[b_RA])
        S.dma(ident[:], ident_d, writes=[b_ident])
        S.dma(aqn[:], aqn_d, writes=[b_aqn])
        S.dma(akvn[:], akvn_d, writes=[b_akvn])
        S.dma(bqn[:], bqn_d, writes=[b_bqn])
        S.dma(bkn[:], bkn_d, writes=[b_bkn])
        S.dma(lngb[:], ln_d, writes=[b_lngb])
        S.dma(w_r[:], w_r_d.rearrange("(kc p) n -> p kc n", p=128), writes=[b_w_r])
        S.dma(b_r[:], b_r_d, writes=[b_b_r])

        bw = {k: Buf(k, multi=True) for k in ("w_in_b", "w_uq_b", "w_ukv_b", "w_ao_b", "w_bo_b", "w_out_b", "e_b")}
        R4 = D // 4
        for i in range(4):
            S.dma(w_in_b[i * R4:(i + 1) * R4, :], w_in_d[i * R4:(i + 1) * R4, :], writes=[bw["w_in_b"]], q="pool")
        S.dma(w_uq_b, w_uq_d, writes=[bw["w_uq_b"]], q="pool")
        S.dma(w_ukv_b, w_ukv_d, writes=[bw["w_ukv_b"]], q="pool")
        S.dma(w_ao_b, w_ao_d, writes=[bw["w_ao_b"]], q="pool")
        S.dma(w_bo_b, w_bo_d, writes=[bw["w_bo_b"]], q="pool")
        S.dma(w_out_b, w_out_d, writes=[bw["w_out_b"]], q="pool")

        with ExitStack() as es0:
            cs, b_cs = sb(es0, "cs", [128, KC, 2], F32)
            badaT, b_badaT = sb(es0, "badaT", [128, 96], F32)
            wa = [sb(es0, f"wa{i}", [128, KC, 128], F32) for i in range(3)]
            S.dma(cs[:], cT_d, writes=[b_cs])
            S.dma(badaT[:], b_ada_d, writes=[b_badaT])
            S.op(ACT, lambda: nc.scalar.activation(out=cs[:], in_=cs[:], func=AF.Silu), reads=[b_cs], writes=[b_cs])
            wav = w_ada_d.rearrange("(kc p) n -> p kc n", p=128)
            for j in range(96 + 2):
                if j < 96:
                    t_, b_ = wa[j % 3]
                    S.dma(t_[:], wav[:, :, j * 128:(j + 1) * 128], writes=[b_])
                jj = j - 2
                if jj >= 0:
                    t_, b_ = wa[jj % 3]
                    pi = gp()
                    for kc in range(KC):
                        S.op(PE, lambda t_=t_, kc=kc, pi=pi: nc.tensor.matmul(
                            ps[pi][:, 0:2], t_[:, kc, :], cs[:, kc, :], start=(kc == 0), stop=(kc == KC - 1)),
                            reads=[b_, b_cs], writes=[bps[pi]], signal=(kc == KC - 1))
                    S.op(DVE, lambda jj=jj, pi=pi: nc.vector.tensor_scalar(
                        out=mod[:, jj, :], in0=ps[pi][:, 0:2], scalar1=badaT[:, jj:jj + 1], scalar2=None, op0=ALU.add),
                        reads=[bps[pi], b_badaT], writes=[b_mod])
            for lo in (16, 64):
                S.op(DVE, lambda lo=lo: nc.vector.tensor_scalar(
                    out=mod[:, lo:lo + 16, :], in0=mod[:, lo:lo + 16, :], scalar1=1.0, scalar2=None, op0=ALU.add),
                    reads=[b_mod], writes=[b_mod])
        S.barrier()

        cast_list = []
        for e in range(NE):
            cast_list += [(eg_b[e], eg_d[e]), (eu_b[e], eu_d[e]), (ed_b[e], ed_d[e])]
        n_a_tiles = (NKP + NKS) // TQ
        casts_per_tile = -(-len(cast_list) // n_a_tiles)

        def emit_casts(n):
            for _ in range(n):
                if cast_list:
                    d_, s_ = cast_list.pop(0)
                    S.dma(d_, s_, writes=[bw["e_b"]], q="pool")

        SH1, SC1, G1, SH2, SC2, G2 = 0, 16, 32, 48, 64, 80
        bKV = [Buf("kv0", multi=True), Buf("kv1", multi=True)]
        b_x1n_s, b_h2_s, b_cw_s = Buf("x1n_s", multi=True), Buf("h2_s", multi=True), Buf("cw_s", multi=True)

        with ExitStack() as es1:
            xT, b_xT = sb(es1, "xT", [128, KC, TQ], F32)
            hT, b_hT = sb(es1, "hT", [128, KC, TQ], BF16)
            qAn, b_qAn = sb(es1, "qAn", [128, NH, TQ], BF16)
            qAr, b_qAr = sb(es1, "qAr", [128, NH, TQ], BF16)
            qB, b_qB = sb(es1, "qB", [128, NH, TQ], BF16)
            bq_heads = {"qAn": [Buf(f"qAn{h}") for h in range(NH)], "qAr": [Buf(f"qAr{h}") for h in range(NH)],
                        "qB": [Buf(f"qB{h}") for h in range(NH)]}
            NWS = 3
            wslot = [sb(es1, f"wslot{i}", [128, KC, 256], BF16) for i in range(NWS)]
            ws_state = [0]
            KCH = 1024
            NKS_ = 3
            kts = [sb(es1, f"kt{i}", [128, KCH], BF16) for i in range(NKS_)]
            krs = [sb(es1, f"kr{i}", [128, KCH], BF16) for i in range(NKS_)]
            for t_, b_ in krs:
                S.op(DVE, lambda t_=t_: nc.vector.memset(t_[:], 0.0), writes=[b_])
            vvs = [sb(es1, f"vv{i}", [128, KCH // 128, 128], BF16) for i in range(NKS_)]
            pTs = [sb(es1, f"pT{i}", [128, 512], BF16) for i in range(4)]
            paccs = [[sb(es1, f"pacc{a}_{i}", [128, 512], F32) for i in range(3)] for a in range(2)]
            craw, b_craw = sb(es1, "craw", [128, 4, TQ], F32)
            cn, b_cn = sb(es1, "cn", [128, 4, TQ], BF16)
            tcA, b_tcA = sb(es1, "tcA", [64, 2, TQ], F32)
            tcB, b_tcB = sb(es1, "tcB", [128, 2, TQ], F32)
            rstd, b_rstd = sb(es1, "rstd", [128, TQ], F32)
            mean, b_mean = sb(es1, "mean", [128, TQ], F32)
            tmp32 = [sb(es1, f"tmp32_{i}", [128, TQ], F32) for i in range(5)]
            tmp16 = [sb(es1, f"tmp16_{i}", [128, TQ], BF16) for i in range(4)]
            t32_state = [0]
            t16_state = [0]
            lg, b_lg = sb(es1, "lg", [128, 72], F32)
            sm = [sb(es1, f"sm{i}", [128, 8], F32) for i in range(8)]
            sc = [sb(es1, f"sc{i}", [128, 1], F32) for i in range(10)]
            cw, b_cw = sb(es1, "cw", [128, NE], F32)
            cwT, b_cwT = sb(es1, "cwT", [64, TQ], F32)

            def t32():
                i = t32_state[0] % len(tmp32)
                t32_state[0] += 1
                return tmp32[i]

            def t16():
                i = t16_state[0] % len(tmp16)
                t16_state[0] += 1
                return tmp16[i]

            def load_w(src, bsrc, krows, c0, ncols):
                i = ws_state[0] % NWS
                ws_state[0] += 1
                t_, b_ = wslot[i]
                kcn = krows // 128
                S.dma(t_[:, 0:kcn, 0:ncols], src.rearrange("(kc p) n -> p kc n", p=128)[:, :, c0:c0 + ncols],
                      reads=[bsrc], writes=[b_])
                return t_, b_

            def run_blocks(specs, pre=2):
                loaded = []
                for i in range(len(specs) + pre):
                    if i < len(specs):
                        sp_ = specs[i]
                        loaded.append(load_w(sp_[0], sp_[1], sp_[2], sp_[3], sp_[4]))
                    j = i - pre
                    if j >= 0:
                        specs[j][5](*loaded[j])

            def proj(pi, wt, bwt, kcn, c0, m, rhs_fn, rhs_bufs, prow=128):
                for kc in range(kcn):
                    S.op(PE, lambda kc=kc: nc.tensor.matmul(
                        ps[pi][0:m, :], wt[0:prow, kc, c0:c0 + m], rhs_fn(kc), start=(kc == 0), stop=(kc == kcn - 1)),
                        reads=[bwt] + rhs_bufs, writes=[bps[pi]], signal=(kc == kcn - 1))

            def load_x_tile(src, t0, job):
                S.dma(xT[:], src.rearrange("(kc p) n -> p kc n", p=128)[:, :, t0:t0 + TQ], writes=[b_xT])
                for kc in range(KC):
                    S.op(DVE, lambda kc=kc: nc.vector.tensor_scalar(
                        out=hT[:, kc, :], in0=xT[:, kc, :], scalar1=mod[:, SC1 + kc, job:job + 1],
                        scalar2=mod[:, SH1 + kc, job:job + 1], op0=ALU.mult, op1=ALU.add),
                        reads=[b_xT, b_mod], writes=[b_hT])

            def load_tables(tA, tB, t0):
                S.dma(tcA[:], tA.rearrange("c p n -> p c n")[:, :, t0:t0 + TQ], writes=[b_tcA])
                S.dma(tcB[:], tB.rearrange("c p n -> p c n")[:, :, t0:t0 + TQ], writes=[b_tcB])

            def rms_lowrank(specs_cols, gain, c_off):
                def consume(blk):
                    def f(wt, bwt):
                        for cc in range(2):
                            c = blk * 2 + cc
                            pi = gp()
                            proj(pi, wt, bwt, KC, cc * 128, 128, lambda kc: hT[:, kc, :], [b_hT])
                            S.op(ACT, lambda: nc.scalar.copy(out=craw[:, c, :], in_=ps[pi][:]),
                                 reads=[bps[pi]], writes=[b_craw])
                            sq, bsq = t16()
                            S.op(ACT, lambda: nc.scalar.activation(out=sq[:], in_=ps[pi][:], func=AF.Square),
                                 reads=[bps[pi]], writes=[bsq])
                            S.op(PE, lambda: nc.tensor.matmul(ps[6][:], ones_bf[:], sq[:], start=(c == 0), stop=(c == 3)),
                                 reads=[b_ones, bsq], writes=[bps[6]], signal=True)
                    return f
                run_blocks([(w_in_b, bw["w_in_b"], D, c_off + blk * 256, 256, consume(blk)) for blk in range(2)])
                S.op(ACT, lambda: nc.scalar.activation(out=rstd[:], in_=ps[6][:], func=AF.Sqrt, bias=epsr[:], scale=1.0 / 512),
                     reads=[bps[6], b_epsr], writes=[b_rstd])
                S.op(DVE, lambda: nc.vector.reciprocal(out=rstd[:], in_=rstd[:]), reads=[b_rstd], writes=[b_rstd])
                for c in range(4):
                    S.op(DVE, lambda c=c: nc.vector.scalar_tensor_tensor(
                        out=cn[:, c, :], in0=craw[:, c, :], scalar=gain[0][:, c:c + 1], in1=rstd[:], op0=ALU.mult, op1=ALU.mult),
                        reads=[b_craw, gain[1], b_rstd], writes=[b_cn])

            def rope_finish(pi_raw, raw16, braw16, R_bf, bR, tab, btab, rows, out_ap, out_bufs):
                pj = gp()
                S.op(PE, lambda: nc.tensor.matmul(ps[pj][0:rows, :], R_bf[:], raw16[0:rows, :], start=True, stop=True),
                     reads=[bR, braw16], writes=[bps[pj]])
                ta, bta = t32()
                tb, btb = t32()
                S.op(DVE, lambda: nc.vector.tensor_tensor(out=ta[0:rows, :], in0=raw16[0:rows, :], in1=tab[0:rows, 0, :], op=ALU.mult),
                     reads=[braw16, btab], writes=[bta])
                S.op(DVE, lambda: nc.vector.tensor_tensor(out=tb[0:rows, :], in0=ps[pj][0:rows, :], in1=tab[0:rows, 1, :], op=ALU.mult),
                     reads=[bps[pj], btab], writes=[btb])
                S.op(DVE, lambda: nc.vector.tensor_tensor(out=out_ap, in0=ta[0:rows, :], in1=tb[0:rows, :], op=ALU.add),
                     reads=[bta, btb], writes=out_bufs)

            def headnorm_rope(pi, gainb, out_ap, out_bufs):
                sq, bsq = t16()
                S.op(ACT, lambda: nc.scalar.activation(out=sq[:], in_=ps[pi][:], func=AF.Square), reads=[bps[pi]], writes=[bsq])
                pj = gp()
                S.op(PE, lambda: nc.tensor.matmul(ps[pj][:], ones_bf[:], sq[:], start=True, stop=True),
                     reads=[b_ones, bsq], writes=[bps[pj]])
                rs, brs = t32()
                S.op(ACT, lambda: nc.scalar.activation(out=rs[:], in_=ps[pj][:], func=AF.Sqrt, bias=epsr[:], scale=1.0 / 128),
                     reads=[bps[pj], b_epsr], writes=[brs])
                S.op(DVE, lambda: nc.vector.reciprocal(out=rs[:], in_=rs[:]), reads=[brs], writes=[brs])
                kn, bkn_ = t16()
                S.op(DVE, lambda: nc.vector.scalar_tensor_tensor(
                    out=kn[:], in0=ps[pi][:], scalar=gainb[0][:, 0:1], in1=rs[:], op0=ALU.mult, op1=ALU.mult),
                    reads=[bps[pi], gainb[1], brs], writes=[bkn_])
                rope_finish(pi, kn, bkn_, RB_bf, b_RB, tcB, b_tcB, 128, out_ap, out_bufs)

            def phase_a(job):
                nk = NK[job]
                for t in range(nk // TQ):
                    t0 = t * TQ
                    emit_casts(casts_per_tile)
                    load_x_tile(xk[job], t0, job)
                    load_tables(tAk[job], tBk[job], t0)
                    rms_lowrank(None, (akvn, b_akvn), C_CKV)

                    specs = []

                    def kr_consume(wt, bwt):
                        pi = gp()
                        proj(pi, wt, bwt, KC, 0, 64, lambda kc: hT[:, kc, :], [b_hT])
                        r16, br16 = t16()
                        S.op(ACT, lambda: nc.scalar.copy(out=r16[0:64, :], in_=ps[pi][0:64, :]), reads=[bps[pi]], writes=[br16])
                        o16, bo16 = t16()
                        rope_finish(pi, r16, br16, RA_bf, b_RA, tcA, b_tcA, 64, o16[0:64, :], [bo16])
                        S.dma(KR[job][:, t0:t0 + TQ], o16[0:64, :], reads=[bo16], writes=[bKV[job]])
                    specs.append((w_in_b, bw["w_in_b"], D, C_KR, 64, kr_consume))

                    def kb_consume(blk):
                        def f(wt, bwt):
                            for cc in range(2):
                                kvh = blk * 2 + cc
                                pi = gp()
                                proj(pi, wt, bwt, KC, cc * 128, 128, lambda kc: hT[:, kc, :], [b_hT])
                                o16, bo16 = t16()
                                headnorm_rope(pi, (bkn, b_bkn), o16[:], [bo16])
                                S.dma(KB[job][kvh, :, t0:t0 + TQ], o16[:], reads=[bo16], writes=[bKV[job]])
                        return f
                    for blk in range(2):
                        specs.append((w_in_b, bw["w_in_b"], D, C_KB + blk * 256, 256, kb_consume(blk)))

                    def vb_consume(blk):
                        def f(wt, bwt):
                            for half in range(2):
                                pi = gp()
                                for s2 in range(2):
                                    sub = half * 2 + s2
                                    for kc in range(KC):
                                        S.op(PE, lambda kc=kc, sub=sub, s2=s2: nc.tensor.matmul(
                                            ps[pi][:, s2 * 256:(s2 + 1) * 256], hT[:, kc, sub * 128:(sub + 1) * 128],
                                            wt[:, kc, 0:256], start=(kc == 0), stop=(kc == KC - 1)),
                                            reads=[bwt, b_hT], writes=[bps[pi]], signal=(kc == KC - 1 and s2 == 1))
                                o16, bo16 = t16()
                                S.op(ACT, lambda: nc.scalar.copy(out=o16[:], in_=ps[pi][:]), reads=[bps[pi]], writes=[bo16])
                                for s2 in range(2):
                                    sub = half * 2 + s2
                                    for hh in range(2):
                                        kvh = blk * 2 + hh
                                        S.dma(VB[job][kvh, t0 + sub * 128:t0 + (sub + 1) * 128, :],
                                              o16[:, s2 * 256 + hh * 128:s2 * 256 + (hh + 1) * 128], reads=[bo16], writes=[bKV[job]])
                        return f
                    for blk in range(2):
                        specs.append((w_in_b, bw["w_in_b"], D, C_VB + blk * 256, 256, vb_consume(blk)))

                    def ukv_consume(h):
                        def f(wt, bwt):
                            pi = gp()
                            proj(pi, wt, bwt, 4, 0, 128, lambda kc: cn[:, kc, :], [b_cn])
                            o16, bo16 = t16()
                            S.op(ACT, lambda: nc.scalar.copy(out=o16[:], in_=ps[pi][:]), reads=[bps[pi]], writes=[bo16])
                            S.dma(KA[job][h, :, t0:t0 + TQ], o16[:], reads=[bo16], writes=[bKV[job]])
                            pj = gp()
                            for sub in range(4):
                                for kc in range(4):
                                    S.op(PE, lambda kc=kc, sub=sub: nc.tensor.matmul(
                                        ps[pj][:, sub * 128:(sub + 1) * 128], cn[:, kc, sub * 128:(sub + 1) * 128],
                                        wt[:, kc, 128:256], start=(kc == 0), stop=(kc == 3)),
                                        reads=[bwt, b_cn], writes=[bps[pj]], signal=(kc == 3 and sub == 3))
                            v16, bv16 = t16()
                            S.op(DVE, lambda: nc.vector.tensor_copy(v16[:], ps[pj][:]), reads=[bps[pj]], writes=[bv16])
                            S.dma(VA[job][h, t0:t0 + TQ, :].rearrange("(s p) d -> p s d", p=128),
                                  v16[:].rearrange("p (s d) -> p s d", d=128), reads=[bv16], writes=[bKV[job]])
                        return f
                    for h in range(NH):
                        specs.append((w_ukv_b, bw["w_ukv_b"], 512, h * 256, 256, ukv_consume(h)))
                    run_blocks(specs)

            def attn_head(job, kparts, vsrc, q_aps, q_bufs, out_ap, out_bufs, scale, hidx):
                nk = NK[job]
                nch = nk // KCH
                po = 4 + (hidx % 2)
                pz = 6 + (hidx % 2)
                nblk = KCH // 128
                chunk_bufs = {}

                def load_chunk(c):
                    i = attn_state[0] % NKS_
                    attn_state[0] += 1
                    got = []
                    for (ksrc, rows, slots) in kparts:
                        t_, b_ = slots[i]
                        S.dma(t_[0:rows, :], ksrc[:, c * KCH:(c + 1) * KCH], reads=[bKV[job]], writes=[b_])
                        got.append((t_, b_, 128))
                    vt, bvt = vvs[i]
                    S.dma(vt[:], vsrc[c * KCH:(c + 1) * KCH, :].rearrange("(b p) d -> p b d", p=128), reads=[bKV[job]], writes=[bvt])
                    chunk_bufs[c] = (got, vt, bvt)

                items = [(c, kb) for c in range(nch) for kb in range(nblk)]
                n = len(items)
                sbank = {}

                def emit_s(i):
                    c, kb = items[i]
                    got, vt, bvt = chunk_bufs[c]
                    pi = gp()
                    sbank[i] = pi
                    np_ = len(got)
                    for j, (t_, b_, rows) in enumerate(got):
                        S.op(PE, lambda t_=t_, rows=rows, j=j: nc.tensor.matmul(
                            ps[pi][:], t_[0:rows, kb * 128:(kb + 1) * 128], q_aps[j], start=(j == 0), stop=(j == np_ - 1)),
                            reads=[b_] + q_bufs, writes=[bps[pi]], signal=(j == np_ - 1))

                load_chunk(0)
                if nch > 1:
                    load_chunk(1)
                LA = 2
                for i in range(min(LA, n)):
                    emit_s(i)
                for i in range(n):
                    c, kb = items[i]
                    if kb == 0 and c + 2 < nch:
                        load_chunk(c + 2)
                    if i + LA < n:
                        emit_s(i + LA)
                    pi = sbank.pop(i)
                    pt, bpt = pTs[i % 4]
                    S.op(ACT, lambda pt=pt, pi=pi: nc.scalar.activation(out=pt[:], in_=ps[pi][:], func=AF.Exp, scale=scale),
                         reads=[bps[pi]], writes=[bpt])
                    got, vt, bvt = chunk_bufs[c]
                    S.op(PE, lambda vt=vt, pt=pt, kb=kb: nc.tensor.matmul(
                        ps[po][:], vt[:, kb, :], pt[:], start=(i == 0), stop=(i == n - 1)),
                        reads=[bvt, bpt], writes=[bps[po]], signal=True)
                    a3 = i % 3
                    at, bat = paccs[hidx % 2][a3]
                    if a3 == 2:
                        if i < 3:
                            S.op(POOL, lambda at=at, pt=pt: nc.gpsimd.tensor_copy(at[:], pt[:]), reads=[bpt], writes=[bat])
                        else:
                            S.op(POOL, lambda at=at, pt=pt: nc.gpsimd.tensor_tensor(out=at[:], in0=pt[:], in1=at[:], op=ALU.add),
                                 reads=[bpt, bat], writes=[bat])
                    else:
                        if i < 3:
                            S.op(DVE, lambda at=at, pt=pt: nc.vector.tensor_copy(at[:], pt[:]), reads=[bpt], writes=[bat])
                        else:
                            S.op(DVE, lambda at=at, pt=pt: nc.vector.tensor_tensor(out=at[:], in0=pt[:], in1=at[:], op=ALU.add),
                                 reads=[bpt, bat], writes=[bat])
                nacc = min(3, n)
                at0, bat0 = paccs[hidx % 2][0]
                for a3 in range(1, nacc):
                    at, bat = paccs[hidx % 2][a3]
                    S.op(DVE, lambda at=at: nc.vector.tensor_tensor(out=at0[:], in0=at[:], in1=at0[:], op=ALU.add),
                         reads=[bat, bat0], writes=[bat0])
                S.op(PE, lambda: nc.tensor.matmul(ps[pz][:], ones_f[:], at0[:], start=True, stop=True),
                     reads=[b_ones_f, bat0], writes=[bps[pz]], signal=True)
                rs, brs = t32()
                S.op(DVE, lambda: nc.vector.reciprocal(out=rs[:], in_=ps[pz][:]), reads=[bps[pz]], writes=[brs])
                S.op(DVE, lambda: nc.vector.tensor_tensor(out=out_ap, in0=ps[po][:], in1=rs[:], op=ALU.mult),
                     reads=[bps[po], brs], writes=out_bufs)

            attn_state = [0]

            def layer_norm_inplace(X, bX, g_idx, b_idx):
                for dc in range(KC):
                    xb, bxb = t16()
                    S.op(ACT, lambda dc=dc, xb=xb: nc.scalar.copy(out=xb[:], in_=X[:, dc, :]), reads=[bX], writes=[bxb])
                    sq, bsq = t16()
                    S.op(ACT, lambda dc=dc, sq=sq: nc.scalar.activation(out=sq[:], in_=X[:, dc, :], func=AF.Square), reads=[bX], writes=[bsq])
                    S.op(PE, lambda dc=dc, xb=xb: nc.tensor.matmul(ps[4][:], onesD_bf[:], xb[:], start=(dc == 0), stop=(dc == KC - 1)),
                         reads=[b_onesD, bxb], writes=[bps[4]], signal=True)
                    S.op(PE, lambda dc=dc, sq=sq: nc.tensor.matmul(ps[5][:], onesD_bf[:], sq[:], start=(dc == 0), stop=(dc == KC - 1)),
                         reads=[b_onesD, bsq], writes=[bps[5]], signal=True)
                S.op(ACT, lambda: nc.scalar.copy(out=mean[:], in_=ps[4][:]), reads=[bps[4]], writes=[b_mean])
                m2, bm2 = t32()
                S.op(DVE, lambda: nc.vector.tensor_tensor(out=m2[:], in0=mean[:], in1=mean[:], op=ALU.mult), reads=[b_mean], writes=[bm2])
                S.op(DVE, lambda: nc.vector.tensor_tensor(out=rstd[:], in0=ps[5][:], in1=m2[:], op=ALU.subtract), reads=[bps[5], bm2], writes=[b_rstd])
                S.op(ACT, lambda: nc.scalar.activation(out=rstd[:], in_=rstd[:], func=AF.Sqrt, bias=epsl[:], scale=1.0),
                     reads=[b_rstd, b_epsl], writes=[b_rstd])
                S.op(DVE, lambda: nc.vector.reciprocal(out=rstd[:], in_=rstd[:]), reads=[b_rstd], writes=[b_rstd])
                for dc in range(KC):
                    S.op(POOL, lambda dc=dc: nc.gpsimd.tensor_tensor(out=X[:, dc, :], in0=X[:, dc, :], in1=mean[:], op=ALU.subtract),
                         reads=[bX, b_mean], writes=[bX])
                    S.op(DVE, lambda dc=dc: nc.vector.tensor_tensor(out=X[:, dc, :], in0=X[:, dc, :], in1=rstd[:], op=ALU.mult),
                         reads=[bX, b_rstd], writes=[bX])
                    S.op(DVE, lambda dc=dc: nc.vector.tensor_scalar(
                        out=X[:, dc, :], in0=X[:, dc, :], scalar1=lngb[:, g_idx, dc:dc + 1], scalar2=lngb[:, b_idx, dc:dc + 1],
                        op0=ALU.mult, op1=ALU.add), reads=[bX, b_lngb], writes=[bX])

            def phase_b(job, t, tok0):
                t0 = t * TQ
                load_x_tile(xq[job], t0, job)
                load_tables(tAq[job], tBq[job], t0)
                S.op(ACT, lambda: nc.scalar.mul(out=xT[:], in_=xT[:], mul=ALPHA), reads=[b_xT, b_hT], writes=[b_xT])
                rms_lowrank(None, (aqn, b_aqn), C_CQ)
                S.op(DVE, lambda: nc.vector.memset(qAr[64:128, :, :], 0.0), writes=[b_qAr] + bq_heads["qAr"])

                specs = []

                def uq_consume(h):
                    def f(wt, bwt):
                        pi = gp()
                        proj(pi, wt, bwt, 4, 0, 128, lambda kc: cn[:, kc, :], [b_cn])
                        S.op(ACT, lambda: nc.scalar.copy(out=qAn[:, h, :], in_=ps[pi][:]), reads=[bps[pi]], writes=[bq_heads["qAn"][h]])
                        pj = gp()
                        proj(pj, wt, bwt, 4, 128, 64, lambda kc: cn[:, kc, :], [b_cn])
                        r16, br16 = t16()
                        S.op(ACT, lambda: nc.scalar.copy(out=r16[0:64, :], in_=ps[pj][0:64, :]), reads=[bps[pj]], writes=[br16])
                        rope_finish(pj, r16, br16, RA_bf, b_RA, tcA, b_tcA, 64, qAr[0:64, h, :], [bq_heads["qAr"][h]])
                    return f
                for h in range(NH):
                    specs.append((w_uq_b, bw["w_uq_b"], 512, h * 192, 192, uq_consume(h)))

                def qb_consume(blk):
                    def f(wt, bwt):
                        for cc in range(2):
                            h = blk * 2 + cc
                            pi = gp()
                            proj(pi, wt, bwt, KC, cc * 128, 128, lambda kc: hT[:, kc, :], [b_hT])
                            headnorm_rope(pi, (bqn, b_bqn), qB[:, h, :], [bq_heads["qB"][h]])
                    return f
                for blk in range(8):
                    specs.append((w_in_b, bw["w_in_b"], D, C_QB + blk * 256, 256, qb_consume(blk)))
                run_blocks(specs)

                for h in range(NH):
                    attn_head(job, [(KA[job][h], 128, kts), (KR[job], 64, krs)], VA[job][h],
                              [qAn[:, h, :], qAr[:, h, :]], [bq_heads["qAn"][h], bq_heads["qAr"][h]],
                              qAn[:, h, :], [bq_heads["qAn"][h]], 192.0 ** -0.5, h)
                for h in range(NH):
                    kvh = h // 4
                    attn_head(job, [(KB[job][kvh], 128, kts)], VB[job][kvh],
                              [qB[:, h, :]], [bq_heads["qB"][h]], qB[:, h, :], [bq_heads["qB"][h]], 128.0 ** -0.5, h)

                mT, b_mT = qAr, b_qAr
                allA = bq_heads["qAn"] + bq_heads["qAr"]
                specs = []
                ta_keep = {}

                def o_consume(which, dcp):
                    src, bsrc_heads, base = (qAn, bq_heads["qAn"], 4) if which == 0 else (qB, bq_heads["qB"], 6)

                    def f(wt, bwt):
                        for cc in range(2):
                            proj(base + cc, wt, bwt, KC, cc * 128, 128, lambda kc: src[:, kc, :], bsrc_heads)
                    return f

                def g_consume(which, dcp):
                    base = 4 if which == 0 else 6

                    def f(wt, bwt):
                        for cc in range(2):
                            dc = dcp * 2 + cc
                            pi = gp()
                            proj(pi, wt, bwt, KC, cc * 128, 128, lambda kc: hT[:, kc, :], [b_hT])
                            sg, bsg = t32()
                            S.op(ACT, lambda: nc.scalar.activation(out=sg[:], in_=ps[pi][:], func=AF.Sigmoid), reads=[bps[pi]], writes=[bsg])
                            if which == 0:
                                ta, bta = t32()
                                S.op(DVE, lambda: nc.vector.tensor_tensor(out=ta[:], in0=ps[base + cc][:], in1=sg[:], op=ALU.mult),
                                     reads=[bps[base + cc], bsg], writes=[bta])
                                ta_keep[cc] = (ta, bta)
                            else:
                                ta, bta = ta_keep[cc]
                                tb, btb = t32()
                                S.op(DVE, lambda: nc.vector.tensor_tensor(out=tb[:], in0=ps[base + cc][:], in1=sg[:], op=ALU.mult),
                                     reads=[bps[base + cc], bsg], writes=[btb])
                                S.op(DVE, lambda: nc.vector.tensor_tensor(out=mT[:, dc, :], in0=ta[:], in1=tb[:], op=ALU.add),
                                     reads=[bta, btb], writes=[b_mT] + bq_heads["qAr"])
                    return f
                for dcp in range(8):
                    specs.append((w_ao_b, bw["w_ao_b"], D, dcp * 256, 256, o_consume(0, dcp)))
                    specs.append((w_in_b, bw["w_in_b"], D, C_GA + dcp * 256, 256, g_consume(0, dcp)))
                    specs.append((w_bo_b, bw["w_bo_b"], D, dcp * 256, 256, o_consume(1, dcp)))
                    specs.append((w_in_b, bw["w_in_b"], D, C_GB + dcp * 256, 256, g_consume(1, dcp)))

                def wout_consume(dcp):
                    def f(wt, bwt):
                        for cc in range(2):
                            dc = dcp * 2 + cc
                            pi = gp()
                            proj(pi, wt, bwt, KC, cc * 128, 128, lambda kc: mT[:, kc, :], [b_mT] + bq_heads["qAr"])
                            S.op(DVE, lambda: nc.vector.scalar_tensor_tensor(
                                out=xT[:, dc, :], in0=ps[pi][:], scalar=mod[:, G1 + dc, job:job + 1], in1=xT[:, dc, :],
                                op0=ALU.mult, op1=ALU.add), reads=[bps[pi], b_mod, b_xT], writes=[b_xT])
                    return f
                for dcp in range(8):
                    specs.append((w_out_b, bw["w_out_b"], D, dcp * 256, 256, wout_consume(dcp)))
                run_blocks(specs)

                layer_norm_inplace(xT, b_xT, 0, 1)
                S.dma(x1n_s[:, tok0:tok0 + TQ].rearrange("(kc p) n -> p kc n", p=128), xT[:], reads=[b_xT], writes=[b_x1n_s])

                for dc in range(KC):
                    h2f, bh2f = t32()
                    S.op(DVE, lambda dc=dc, h2f=h2f: nc.vector.tensor_scalar(
                        out=h2f[:], in0=xT[:, dc, :], scalar1=mod[:, SC2 + dc, job:job + 1], scalar2=mod[:, SH2 + dc, job:job + 1],
                        op0=ALU.mult, op1=ALU.add), reads=[b_xT, b_mod], writes=[bh2f])
                    S.op(PE, lambda dc=dc, h2f=h2f: nc.tensor.matmul(ps[6][0:72, :], w_r[:, dc, :], h2f[:], start=(dc == 0), stop=(dc == KC - 1)),
                         reads=[b_w_r, bh2f], writes=[bps[6]], signal=True)
                    S.op(ACT, lambda dc=dc, h2f=h2f: nc.scalar.copy(out=hT[:, dc, :], in_=h2f[:]), reads=[bh2f], writes=[b_hT])
                S.dma(h2_s[:, tok0:tok0 + TQ].rearrange("(kc p) n -> p kc n", p=128), hT[:], reads=[b_hT], writes=[b_h2_s])
                lt, blt = t32()
                S.op(ACT, lambda: nc.scalar.copy(out=lt[0:72, :], in_=ps[6][0:72, :]), reads=[bps[6]], writes=[blt])
                pc = 7
                for sub in range(4):
                    pi = gp()
                    S.op(PE, lambda: nc.tensor.transpose(out=ps[pi][:, 0:72], in_=lt[0:72, sub * 128:(sub + 1) * 128], identity=ident[0:72, 0:72]),
                         reads=[blt, b_ident], writes=[bps[pi]])
                    S.op(DVE, lambda: nc.vector.tensor_tensor(out=lg[:], in0=ps[pi][:, 0:72], in1=b_r[:], op=ALU.add),
                         reads=[bps[pi], b_b_r], writes=[b_lg])
                    (gmax, b_gmax), (ngmax, b_ngmax), (gsum, b_gsum), (pgrp, b_pgrp), (m1, b_m1), (m2_, b_m2), \
                        (dd, b_dd), (w1, b_w1), (w2, b_w2), (den, b_den) = sc
                    (ohg, b_ohg), (eg_, b_eg), (ein, b_ein), (oh1, b_oh1), (e2, b_e2), (oh2, b_oh2), (cwe, b_cwe), (junk, b_junk) = sm
                    S.op(DVE, lambda: nc.vector.tensor_reduce(out=gmax[:], in_=lg[:, 0:8], axis=AX.X, op=ALU.max), reads=[b_lg], writes=[b_gmax])
                    S.op(DVE, lambda: nc.vector.tensor_scalar(out=ohg[:], in0=lg[:, 0:8], scalar1=gmax[:, 0:1], scalar2=None, op0=ALU.is_equal),
                         reads=[b_lg, b_gmax], writes=[b_ohg])
                    S.op(DVE, lambda: nc.vector.tensor_scalar(out=ngmax[:], in0=gmax[:], scalar1=-1.0, scalar2=None, op0=ALU.mult),
                         reads=[b_gmax], writes=[b_ngmax])
                    S.op(DVE, lambda: nc.vector.memset(gsum[:], 0.0), writes=[b_gsum])
                    S.op(ACT, lambda: nc.scalar.activation(out=eg_[:], in_=lg[:, 0:8], func=AF.Exp, bias=ngmax[:], scale=1.0, accum_out=gsum[:]),
                         reads=[b_lg, b_ngmax], writes=[b_eg, b_gsum])
                    S.op(DVE, lambda: nc.vector.reciprocal(out=pgrp[:], in_=gsum[:]), reads=[b_gsum], writes=[b_pgrp])
                    for g in range(8):
                        if g == 0:
                            S.op(DVE, lambda: nc.vector.tensor_scalar(out=ein[:], in0=lg[:, 8:16], scalar1=ohg[:, 0:1], scalar2=None, op0=ALU.mult),
                                 reads=[b_lg, b_ohg], writes=[b_ein])
                        else:
                            S.op(DVE, lambda g=g: nc.vector.scalar_tensor_tensor(
                                out=ein[:], in0=lg[:, 8 + g * 8:16 + g * 8], scalar=ohg[:, g:g + 1], in1=ein[:], op0=ALU.mult, op1=ALU.add),
                                reads=[b_lg, b_ohg, b_ein], writes=[b_ein])
                    S.op(DVE, lambda: nc.vector.tensor_reduce(out=m1[:], in_=ein[:], axis=AX.X, op=ALU.max), reads=[b_ein], writes=[b_m1])
                    S.op(DVE, lambda: nc.vector.tensor_scalar(out=oh1[:], in0=ein[:], scalar1=m1[:, 0:1], scalar2=None, op0=ALU.is_equal),
                         reads=[b_ein, b_m1], writes=[b_oh1])
                    S.op(DVE, lambda: nc.vector.scalar_tensor_tensor(out=e2[:], in0=oh1[:], scalar=-1e30, in1=ein[:], op0=ALU.mult, op1=ALU.add),
                         reads=[b_oh1, b_ein], writes=[b_e2])
                    S.op(DVE, lambda: nc.vector.tensor_reduce(out=m2_[:], in_=e2[:], axis=AX.X, op=ALU.max), reads=[b_e2], writes=[b_m2])
                    S.op(DVE, lambda: nc.vector.tensor_scalar(out=oh2[:], in0=e2[:], scalar1=m2_[:, 0:1], scalar2=None, op0=ALU.is_equal),
                         reads=[b_e2, b_m2], writes=[b_oh2])
                    S.op(DVE, lambda: nc.vector.tensor_tensor(out=dd[:], in0=m2_[:], in1=m1[:], op=ALU.subtract), reads=[b_m1, b_m2], writes=[b_dd])
                    S.op(ACT, lambda: nc.scalar.activation(out=dd[:], in_=dd[:], func=AF.Exp), reads=[b_dd], writes=[b_dd])
                    S.op(DVE, lambda: nc.vector.tensor_scalar(out=den[:], in0=dd[:], scalar1=1.0, scalar2=None, op0=ALU.add), reads=[b_dd], writes=[b_den])
                    S.op(DVE, lambda: nc.vector.reciprocal(out=w1[:], in_=den[:]), reads=[b_den], writes=[b_w1])
                    S.op(DVE, lambda: nc.vector.tensor_tensor(out=w2[:], in0=dd[:], in1=w1[:], op=ALU.mult), reads=[b_dd, b_w1], writes=[b_w2])
                    S.op(DVE, lambda: nc.vector.tensor_tensor(out=w1[:], in0=w1[:], in1=pgrp[:], op=ALU.mult), reads=[b_w1, b_pgrp], writes=[b_w1])
                    S.op(DVE, lambda: nc.vector.tensor_tensor(out=w2[:], in0=w2[:], in1=pgrp[:], op=ALU.mult), reads=[b_w2, b_pgrp], writes=[b_w2])
                    S.op(DVE, lambda: nc.vector.tensor_scalar(out=cwe[:], in0=oh1[:], scalar1=w1[:, 0:1], scalar2=None, op0=ALU.mult),
                         reads=[b_oh1, b_w1], writes=[b_cwe])
                    S.op(DVE, lambda: nc.vector.scalar_tensor_tensor(out=cwe[:], in0=oh2[:], scalar=w2[:, 0:1], in1=cwe[:], op0=ALU.mult, op1=ALU.add),
                         reads=[b_oh2, b_w2, b_cwe], writes=[b_cwe])
                    for g in range(8):
                        S.op(DVE, lambda g=g: nc.vector.tensor_scalar(out=cw[:, g * 8:(g + 1) * 8], in0=cwe[:], scalar1=ohg[:, g:g + 1], scalar2=None, op0=ALU.mult),
                             reads=[b_cwe, b_ohg], writes=[b_cw])
                    S.op(PE, lambda: nc.tensor.transpose(out=ps[pc][0:64, sub * 128:(sub + 1) * 128], in_=cw[:], identity=ident[:]),
                         reads=[b_cw, b_ident], writes=[bps[pc]])
                S.op(ACT, lambda: nc.scalar.copy(out=cwT[:], in_=ps[pc][0:64, :]), reads=[bps[pc]], writes=[b_cwT])
                S.dma(cw_s[:, tok0:tok0 + TQ], cwT[:], reads=[b_cwT], writes=[b_cw_s])

            tok0 = 0
            tiles = []
            for job in range(2):
                phase_a(job)
            emit_casts(len(cast_list))
            for job in range(2):
                for t in range(NQ[job] // TQ):
                    phase_b(job, t, tok0)
                    tiles.append((job, t, tok0))
                    tok0 += TQ
        S.barrier()

        with ExitStack() as es2:
            h2m, b_h2m = sb(es2, "h2m", [128, KC, TQ], BF16)
            acc, b_acc = sb(es2, "acc", [128, KC, TQ], F32)
            wg = [sb(es2, f"wg{i}", [128, KC, DE], BF16) for i in range(2)]
            wu = [sb(es2, f"wu{i}", [128, KC, DE], BF16) for i in range(2)]
            wd = [sb(es2, f"wd{i}", [128, 4, D], BF16) for i in range(2)]
            hid, b_hid = sb(es2, "hid", [128, 4, TQ], BF16)
            cwb = [sb(es2, f"cwb{i}", [128, TQ], F32) for i in range(3)]
            sgs = [sb(es2, f"sgs{i}", [128, TQ], F32) for i in range(3)]
            xa = [sb(es2, f"xa{i}", [128, TQ], F32) for i in range(2)]
            rstd2, b_rstd2 = sb(es2, "rstd2", [128, TQ], F32)
            mean2, b_mean2 = sb(es2, "mean2", [128, TQ], F32)
            m2t, b_m2t = sb(es2, "m2t", [128, TQ], F32)
            l16 = [sb(es2, f"l16_{i}", [128, TQ], BF16) for i in range(4)]

            hid2, b_hid2 = sb(es2, "hid2", [128, 4, TQ], BF16)
            hids = [(hid, b_hid), (hid2, b_hid2)]

            def load_gu(e, tok0, k):
                i = k % 2
                S.dma(wg[i][0][:], eg_b[e].rearrange("(kc p) n -> p kc n", p=128), reads=[bw["e_b"]], writes=[wg[i][1]])
                S.dma(wu[i][0][:], eu_b[e].rearrange("(kc p) n -> p kc n", p=128), reads=[bw["e_b"]], writes=[wu[i][1]])
                S.dma(cwb[k % 3][0][:], cw_s[e:e + 1, tok0:tok0 + TQ].partition_broadcast(128), reads=[b_cw_s], writes=[cwb[k % 3][1]])

            def load_d(e, k):
                i = k % 2
                S.dma(wd[i][0][:], ed_b[e].rearrange("(kc p) n -> p kc n", p=128), reads=[bw["e_b"]], writes=[wd[i][1]])

            def gate_up(k):
                i = k % 2
                wgt, bwg = wg[i]
                wut, bwu = wu[i]
                cwt_, bcw = cwb[k % 3]
                hd, bhd = hids[i]
                for hc in range(4):
                    pa = gp()
                    for kc in range(KC):
                        S.op(PE, lambda kc=kc: nc.tensor.matmul(ps[pa][:], wgt[:, kc, hc * 128:(hc + 1) * 128], h2m[:, kc, :],
                                                               start=(kc == 0), stop=(kc == KC - 1)),
                             reads=[bwg, b_h2m], writes=[bps[pa]], signal=(kc == KC - 1))
                    pb = gp()
                    for kc in range(KC):
                        S.op(PE, lambda kc=kc: nc.tensor.matmul(ps[pb][:], wut[:, kc, hc * 128:(hc + 1) * 128], h2m[:, kc, :],
                                                               start=(kc == 0), stop=(kc == KC - 1)),
                             reads=[bwu, b_h2m], writes=[bps[pb]], signal=(kc == KC - 1))
                    sg, bsg = sgs[(k * 4 + hc) % 3]
                    S.op(ACT, lambda: nc.scalar.activation(out=sg[:], in_=ps[pa][:], func=AF.Silu), reads=[bps[pa]], writes=[bsg])
                    S.op(POOL, lambda: nc.gpsimd.tensor_tensor(out=sg[:], in0=sg[:], in1=cwt_[:], op=ALU.mult), reads=[bsg, bcw], writes=[bsg])
                    S.op(DVE, lambda: nc.vector.tensor_tensor(out=hd[:, hc, :], in0=ps[pb][:], in1=sg[:], op=ALU.mult),
                         reads=[bps[pb], bsg], writes=[bhd])

            def down(k, first):
                i = k % 2
                wdt, bwd = wd[i]
                hd, bhd = hids[i]
                for dc in range(KC):
                    pd = 4 + (dc % 4)
                    for kc in range(4):
                        S.op(PE, lambda kc=kc: nc.tensor.matmul(ps[pd][:], wdt[:, kc, dc * 128:(dc + 1) * 128], hd[:, kc, :],
                                                               start=(kc == 0), stop=(kc == 3)),
                             reads=[bwd, bhd], writes=[bps[pd]], signal=(kc == 3))
                    if first:
                        S.op(DVE, lambda: nc.vector.tensor_copy(acc[:, dc, :], ps[pd][:]), reads=[bps[pd]], writes=[b_acc])
                    else:
                        S.op(DVE, lambda: nc.vector.tensor_tensor(out=acc[:, dc, :], in0=ps[pd][:], in1=acc[:, dc, :], op=ALU.add),
                             reads=[bps[pd], b_acc], writes=[b_acc])

            k = 0
            for (job, t, tok0) in tiles:
                S.dma(h2m[:], h2_s[:, tok0:tok0 + TQ].rearrange("(kc p) n -> p kc n", p=128), reads=[b_h2_s], writes=[b_h2m])
                load_gu(0, tok0, k)
                load_d(0, k)
                for e in range(NE):
                    if e + 1 < NE:
                        load_gu(e + 1, tok0, k + 1)
                    gate_up(k)
                    if e > 0:
                        down(k - 1, e - 1 == 0)
                    if e + 1 < NE:
                        load_d(e + 1, k + 1)
                    k += 1
                down(k - 1, False)
                for dc in range(KC):
                    xt_, bxt = xa[dc % 2]
                    S.dma(xt_[:], x1n_s[dc * 128:(dc + 1) * 128, tok0:tok0 + TQ], reads=[b_x1n_s], writes=[bxt])
                    S.op(DVE, lambda: nc.vector.tensor_scalar(out=acc[:, dc, :], in0=acc[:, dc, :], scalar1=mod[:, G2 + dc, job:job + 1],
                                                              scalar2=None, op0=ALU.mult), reads=[b_acc, b_mod], writes=[b_acc])
                    S.op(DVE, lambda: nc.vector.scalar_tensor_tensor(out=acc[:, dc, :], in0=xt_[:], scalar=ALPHA, in1=acc[:, dc, :],
                                                                     op0=ALU.mult, op1=ALU.add), reads=[bxt, b_acc], writes=[b_acc])
                for dc in range(KC):
                    xb, bxb = l16[(2 * dc) % 4]
                    sq, bsq = l16[(2 * dc + 1) % 4]
                    S.op(ACT, lambda: nc.scalar.copy(out=xb[:], in_=acc[:, dc, :]), reads=[b_acc], writes=[bxb])
                    S.op(ACT, lambda: nc.scalar.activation(out=sq[:], in_=acc[:, dc, :], func=AF.Square), reads=[b_acc], writes=[bsq])
                    S.op(PE, lambda: nc.tensor.matmul(ps[0][:], onesD_bf[:], xb[:], start=(dc == 0), stop=(dc == KC - 1)),
                         reads=[b_onesD, bxb], writes=[bps[0]], signal=True)
                    S.op(PE, lambda: nc.tensor.matmul(ps[1][:], onesD_bf[:], sq[:], start=(dc == 0), stop=(dc == KC - 1)),
                         reads=[b_onesD, bsq], writes=[bps[1]], signal=True)
                S.op(ACT, lambda: nc.scalar.copy(out=mean2[:], in_=ps[0][:]), reads=[bps[0]], writes=[b_mean2])
                S.op(DVE, lambda: nc.vector.tensor_tensor(out=m2t[:], in0=mean2[:], in1=mean2[:], op=ALU.mult), reads=[b_mean2], writes=[b_m2t])
                S.op(DVE, lambda: nc.vector.tensor_tensor(out=rstd2[:], in0=ps[1][:], in1=m2t[:], op=ALU.subtract), reads=[bps[1], b_m2t], writes=[b_rstd2])
                S.op(ACT, lambda: nc.scalar.activation(out=rstd2[:], in_=rstd2[:], func=AF.Sqrt, bias=epsl[:], scale=1.0),
                     reads=[b_rstd2, b_epsl], writes=[b_rstd2])
                S.op(DVE, lambda: nc.vector.reciprocal(out=rstd2[:], in_=rstd2[:]), reads=[b_rstd2], writes=[b_rstd2])
                for dc in range(KC):
                    S.op(POOL, lambda: nc.gpsimd.tensor_tensor(out=acc[:, dc, :], in0=acc[:, dc, :], in1=mean2[:], op=ALU.subtract),
                         reads=[b_acc, b_mean2], writes=[b_acc])
                    S.op(DVE, lambda: nc.vector.tensor_tensor(out=acc[:, dc, :], in0=acc[:, dc, :], in1=rstd2[:], op=ALU.mult),
                         reads=[b_acc, b_rstd2], writes=[b_acc])
                    S.op(DVE, lambda: nc.vector.tensor_scalar(out=acc[:, dc, :], in0=acc[:, dc, :], scalar1=lngb[:, 2, dc:dc + 1],
                                                              scalar2=lngb[:, 3, dc:dc + 1], op0=ALU.mult, op1=ALU.add),
                         reads=[b_acc, b_lngb], writes=[b_acc])
                b_y = Buf("y")
                S.dma(y_d[job][:, t * TQ:(t + 1) * TQ].rearrange("(kc p) n -> p kc n", p=128), acc[:], reads=[b_acc], writes=[b_y])
                S._waits(S.SP, [b_y], [])
        S.barrier()
    return nc


def _rope_tables(S):
    inv = (THETA ** (-np.arange(0, 64, 2, dtype=np.float32) / np.float32(64))).astype(np.float32)
    t = np.arange(S, dtype=np.int32)
    row = (t // 64).astype(np.float32)
    col = (t % 64).astype(np.float32)
    ang_a = t.astype(np.float32)[:, None] * inv[None, :]
    ang_r = row[:, None] * inv[None, :]
    ang_c = col[:, None] * inv[None, :]
    tA = np.empty((2, 64, S), np.float32)
    tB = np.empty((2, 128, S), np.float32)
    for k, fn in enumerate((np.cos, np.sin)):
        a = fn(ang_a).astype(np.float32).T
        tA[k, 0:32] = a
        tA[k, 32:64] = a
        r = fn(ang_r).astype(np.float32).T
        c = fn(ang_c).astype(np.float32).T
        tB[k, 0:32] = r
        tB[k, 32:64] = r
        tB[k, 64:96] = c
        tB[k, 96:128] = c
    return tA, tB


def _fm(v, kc):
    return np.ascontiguousarray(np.asarray(v, np.float32).reshape(kc, 128).T)


def run(inputs, n_prompt, s_prompt, s_sample):
    f = lambda k: np.asarray(inputs[k], np.float32)
    xp, xs = f("x_prompt"), f("x_sample")
    NKP, NKS = s_prompt, s_sample
    NQP, NQS = s_prompt // 2, s_sample // 8
    nc = build(NKP, NQP, NKS, NQS)

    tAp, tBp = _rope_tables(NKP)
    tAs, tBs = _rope_tables(NKS)
    RA = np.zeros((64, 64), np.float32)
    for i in range(32):
        RA[i + 32, i] = -1.0
        RA[i, i + 32] = 1.0
    RB = np.zeros((128, 128), np.float32)
    RB[0:64, 0:64] = RA
    RB[64:128, 64:128] = RA
    shared = {
        "RA": RA, "RB": RB, "ident": np.eye(128, dtype=np.float32),
        "w_ada": f("w_ada")[0], "b_adaT": _fm(f("b_ada")[0], 96),
        "w_in": f("w_in")[0], "a_q_norm": _fm(f("a_q_norm")[0], 4), "a_kv_norm": _fm(f("a_kv_norm")[0], 4),
        "a_w_uq": f("a_w_uq")[0], "a_w_ukv": f("a_w_ukv")[0], "a_w_o": f("a_w_o")[0],
        "b_q_norm": f("b_q_norm")[0].reshape(128, 1), "b_k_norm": f("b_k_norm")[0].reshape(128, 1),
        "b_w_o": f("b_w_o")[0], "w_out": f("w_out")[0],
        "ln_gb": np.ascontiguousarray(np.stack([_fm(f("ln1_g")[0], 16), _fm(f("ln1_b")[0], 16),
                                                _fm(f("ln2_g")[0], 16), _fm(f("ln2_b")[0], 16)], axis=1)),
        "w_r": np.ascontiguousarray(np.concatenate([f("w_group")[0], f("w_expert")[0]], axis=1)),
        "b_r": np.ascontiguousarray(np.broadcast_to(np.concatenate([f("b_group")[0], f("b_expert")[0]])[None, :], (128, 72))),
        "e_w_gate": f("e_w_gate")[0], "e_w_up": f("e_w_up")[0], "e_w_down": f("e_w_down")[0],
    }
    xpT = [np.ascontiguousarray(xp[b].T) for b in range(n_prompt)]
    xsT = np.ascontiguousarray(xs[0].T)
    cp, cs_ = f("c_prompt"), f("c_sample")
    in_maps = []
    for c in range(8):
        b, half = c // 2, c % 2
        b = min(b, n_prompt - 1)
        cT = np.stack([cp[b], cs_[0]], axis=1)
        cT = np.ascontiguousarray(cT.reshape(KC, 128, 2).transpose(1, 0, 2))
        m = dict(shared)
        m.update({
            "xkp": xpT[b], "xks": xsT,
            "xqp": np.ascontiguousarray(xpT[b][:, half * NQP:(half + 1) * NQP]),
            "xqs": np.ascontiguousarray(xsT[:, c * NQS:(c + 1) * NQS]),
            "tAkp": tAp, "tBkp": tBp, "tAks": tAs, "tBks": tBs,
            "tAqp": np.ascontiguousarray(tAp[:, :, half * NQP:(half + 1) * NQP]),
            "tBqp": np.ascontiguousarray(tBp[:, :, half * NQP:(half + 1) * NQP]),
            "tAqs": np.ascontiguousarray(tAs[:, :, c * NQS:(c + 1) * NQS]),
            "tBqs": np.ascontiguousarray(tBs[:, :, c * NQS:(c + 1) * NQS]),
            "cT": cT,
        })
        in_maps.append(m)
    res = run_bass_kernel_spmd(nc, in_maps, core_ids=list(range(8)))
    y_p = np.empty((n_prompt, s_prompt, D), np.float32)
    y_s = np.empty((1, s_sample, D), np.float32)
    for c in range(8):
        b, half = c // 2, c % 2
        r = res.results[c]
        if b < n_prompt:
            y_p[b, half * NQP:(half + 1) * NQP, :] = np.asarray(r["yp"]).T
        y_s[0, c * NQS:(c + 1) * NQS, :] = np.asarray(r["ys"]).T
    return y_p, y_s


def kernel(**inputs):
    return run(inputs, 4, 8192, 16384)
```
